# Optimizing a Trainium2 kernel written in Bass

```python
import jax, jax.numpy as jnp
from jax import lax
import numpy as np

D_MODEL = 1024
BATCH = 4
SEQ = 4096
DEPTH = 4

POOL_WINDOWS = (2, 4, 8, 16)
POOL_GROUP = 128
POOL_WIDTH = POOL_GROUP * len(POOL_WINDOWS)
ATTN_HEADS = 8
HEAD_DIM = 64
ATTN_WIDTH = ATTN_HEADS * HEAD_DIM
MOBA_BLOCK = 256
MOBA_TOPK = 3
Q_CHUNK = 64
RNN_BLOCKS = 10
RNN_BLOCK = 128
RNN_WIDTH = RNN_BLOCKS * RNN_BLOCK
RNN_CONV = 4
RG_C = 8.0
D_FF = 2816
FFN_CONV = 3
EPS = 1e-6
NEG_INF = -1e30
N_EVEN = (DEPTH + 1) // 2
N_ODD = DEPTH // 2

kernel_name = "hybrid_pool_moba_rglru_convffn"


def rms_norm(x, g):
    x32 = x.astype(jnp.float32)
    y = x32 * lax.rsqrt(jnp.mean(x32 * x32, axis=-1, keepdims=True) + EPS)
    return (y * g.astype(jnp.float32)).astype(x.dtype)


def causal_dwconv(x, w, b):
    K, C = w.shape
    y = lax.conv_general_dilated(
        x, w[:, None, :].astype(x.dtype), window_strides=(1,), padding=[(K - 1, 0)],
        dimension_numbers=('NWC', 'WIO', 'NWC'), feature_group_count=C)
    return y + b.astype(x.dtype)


def multiscale_pool_minus_identity(u):
    S = u.shape[1]
    u32 = u.astype(jnp.float32)
    cs = jnp.cumsum(u32, axis=1)
    count = jnp.arange(1, S + 1, dtype=jnp.float32)[None, :, None]
    outs = []
    for gi, w in enumerate(POOL_WINDOWS):
        sl = slice(gi * POOL_GROUP, (gi + 1) * POOL_GROUP)
        c = cs[..., sl]
        prev = jnp.pad(c, ((0, 0), (w, 0), (0, 0)))[:, :S]
        outs.append((c - prev) / jnp.minimum(count, float(w)) - u32[..., sl])
    return jnp.concatenate(outs, axis=-1)


def head_rms(t, g):
    t32 = t.astype(jnp.float32)
    return t32 * lax.rsqrt(jnp.mean(t32 * t32, axis=-1, keepdims=True) + EPS) * g.astype(jnp.float32)


def moba_attention(q, k, v, q_gain, k_gain):
    B, S, H, Dh = q.shape
    nb = -(-S // MOBA_BLOCK)
    s_pad = nb * MOBA_BLOCK
    pad = ((0, 0), (0, s_pad - S), (0, 0), (0, 0))
    qn = jnp.pad(head_rms(q, q_gain), pad).transpose(0, 2, 1, 3)
    kn = jnp.pad(head_rms(k, k_gain), pad).transpose(0, 2, 1, 3)
    vp = jnp.pad(v.astype(jnp.float32), pad).transpose(0, 2, 1, 3)
    k_blocks = kn.reshape(B, H, nb, MOBA_BLOCK, Dh)
    v_blocks = vp.reshape(B, H, nb, MOBA_BLOCK, Dh)
    slopes = 2.0 ** (-8.0 * jnp.arange(1, H + 1, dtype=jnp.float32) / H)
    scale = Dh ** -0.5
    topk = min(MOBA_TOPK, nb - 1)
    q_blk = jnp.arange(s_pad) // MOBA_BLOCK
    if topk > 0:
        k_mean = jnp.mean(k_blocks, axis=3)
        gate = jnp.einsum('bhqd,bhnd->bhqn', qn, k_mean)
        past = jnp.arange(nb)[None, :] < q_blk[:, None]
        gate = jnp.where(past[None, None], gate, -jnp.inf)
        _, sel_idx = lax.top_k(gate, topk)
        sel_valid = sel_idx < q_blk[None, None, :, None]
    bi = jnp.arange(B)[:, None, None, None]
    hi = jnp.arange(H)[None, :, None, None]
    kpos = jnp.arange(MOBA_BLOCK)

    def chunk(c):
        start = c * Q_CHUNK
        q_c = lax.dynamic_slice_in_dim(qn, start, Q_CHUNK, axis=2)
        t_c = start + jnp.arange(Q_CHUNK)
        own = start // MOBA_BLOCK
        k_own = lax.dynamic_index_in_dim(k_blocks, own, axis=2, keepdims=False)
        v_own = lax.dynamic_index_in_dim(v_blocks, own, axis=2, keepdims=False)
        dist_own = (t_c[:, None] - (own * MOBA_BLOCK + kpos)[None, :]).astype(jnp.float32)
        logit_own = (jnp.einsum('bhqd,bhkd->bhqk', q_c, k_own) * scale
                     - slopes[None, :, None, None] * dist_own[None, None])
        logit_own = jnp.where((dist_own >= 0)[None, None], logit_own, NEG_INF)
        if topk == 0:
            p_own = jax.nn.softmax(logit_own, axis=-1)
            return jnp.einsum('bhqk,bhkd->bhqd', p_own, v_own)
        idx_c = lax.dynamic_slice_in_dim(sel_idx, start, Q_CHUNK, axis=2)
        valid_c = lax.dynamic_slice_in_dim(sel_valid, start, Q_CHUNK, axis=2)
        k_sel = k_blocks[bi, hi, idx_c]
        v_sel = v_blocks[bi, hi, idx_c]
        s_pos = idx_c[..., None] * MOBA_BLOCK + kpos
        dist_sel = (t_c[None, None, :, None, None] - s_pos).astype(jnp.float32)
        logit_sel = (jnp.einsum('bhqd,bhqjkd->bhqjk', q_c, k_sel) * scale
                     - slopes[None, :, None, None, None] * dist_sel)
        logit_sel = jnp.where(valid_c[..., None], logit_sel, NEG_INF)
        n_sel = topk * MOBA_BLOCK
        logits = jnp.concatenate([logit_sel.reshape(B, H, Q_CHUNK, n_sel), logit_own], axis=-1)
        p = jax.nn.softmax(logits, axis=-1)
        p_sel = p[..., :n_sel].reshape(B, H, Q_CHUNK, topk, MOBA_BLOCK)
        p_own = p[..., n_sel:]
        return (jnp.einsum('bhqjk,bhqjkd->bhqd', p_sel, v_sel)
                + jnp.einsum('bhqk,bhkd->bhqd', p_own, v_own))

    outs = lax.map(chunk, jnp.arange(s_pad // Q_CHUNK))
    out = outs.transpose(1, 0, 3, 2, 4).reshape(B, s_pad, H * Dh)
    return out[:, :S]


def pool_attn_mixer(h, w_in, pool_w, pool_scale, q_gain, k_gain, w_out):
    B, S, _ = h.shape
    z = h @ w_in
    u, q, k, v = jnp.split(z, [POOL_WIDTH, POOL_WIDTH + ATTN_WIDTH, POOL_WIDTH + 2 * ATTN_WIDTH], axis=-1)
    p = multiscale_pool_minus_identity(u).astype(h.dtype)
    p = jnp.einsum('bsgc,gcd->bsgd', p.reshape(B, S, len(POOL_WINDOWS), POOL_GROUP), pool_w)
    p = p.reshape(B, S, POOL_WIDTH) * pool_scale
    a = moba_attention(q.reshape(B, S, ATTN_HEADS, HEAD_DIM), k.reshape(B, S, ATTN_HEADS, HEAD_DIM),
                       v.reshape(B, S, ATTN_HEADS, HEAD_DIM), q_gain, k_gain)
    return jnp.concatenate([p, a.astype(h.dtype)], axis=-1) @ w_out


def rglru_mixer(h, w_in, conv_w, conv_b, w_a, b_a, w_x, b_x, lam, w_out):
    B, S, _ = h.shape
    z = h @ w_in
    gate, xr = jnp.split(z, 2, axis=-1)
    xr = causal_dwconv(xr, conv_w, conv_b)
    xb = xr.reshape(B, S, RNN_BLOCKS, RNN_BLOCK)
    r = jax.nn.sigmoid((jnp.einsum('bsnc,ncd->bsnd', xb, w_a).reshape(B, S, RNN_WIDTH) + b_a).astype(jnp.float32))
    i = jax.nn.sigmoid((jnp.einsum('bsnc,ncd->bsnd', xb, w_x).reshape(B, S, RNN_WIDTH) + b_x).astype(jnp.float32))
    log_a = -RG_C * r * jax.nn.softplus(-lam.astype(jnp.float32))
    a = jnp.exp(log_a)
    b = jnp.sqrt(-jnp.expm1(2.0 * log_a)) * (i * xr.astype(jnp.float32))

    def combine(left, right):
        a1, b1 = left
        a2, b2 = right
        return a1 * a2, a2 * b1 + b2

    _, hs = lax.associative_scan(combine, (a, b), axis=1)
    y = jax.nn.gelu(gate) * hs.astype(gate.dtype)
    return y @ w_out


def conv_ffn(h, w_in, conv_w, conv_b, w_out):
    z = causal_dwconv(h @ w_in, conv_w, conv_b)
    u, g = jnp.split(z, 2, axis=-1)
    return (jax.nn.gelu(g) * u) @ w_out


def setup_inputs(seed: int = 0) -> dict:
    key = jax.random.key(seed)
    ks = jax.random.split(key, 24)
    f32 = jnp.float32
    nrm = lambda k, shape, s: jax.random.normal(k, shape, f32) * s
    u = jax.random.uniform(ks[16], (N_ODD, RNN_WIDTH), f32, minval=0.9, maxval=0.999)
    a0 = u ** (1.0 / RG_C)
    return {
        "x": nrm(ks[0], (BATCH, SEQ, D_MODEL), 1.0),
        "norm_mix": 1.0 + nrm(ks[1], (DEPTH, D_MODEL), 0.05),
        "norm_ffn": 1.0 + nrm(ks[2], (DEPTH, D_MODEL), 0.05),
        "pa_w_in": nrm(ks[3], (N_EVEN, D_MODEL, POOL_WIDTH + 3 * ATTN_WIDTH), D_MODEL ** -0.5),
        "pa_pool_w": nrm(ks[4], (N_EVEN, len(POOL_WINDOWS), POOL_GROUP, POOL_GROUP), POOL_GROUP ** -0.5),
        "pa_pool_scale": 1.0 + nrm(ks[5], (N_EVEN, POOL_WIDTH), 0.1),
        "pa_q_gain": 1.0 + nrm(ks[6], (N_EVEN, HEAD_DIM), 0.05),
        "pa_k_gain": 1.0 + nrm(ks[7], (N_EVEN, HEAD_DIM), 0.05),
        "pa_w_out": nrm(ks[8], (N_EVEN, POOL_WIDTH + ATTN_WIDTH, D_MODEL), (POOL_WIDTH + ATTN_WIDTH) ** -0.5),
        "rg_w_in": nrm(ks[9], (N_ODD, D_MODEL, 2 * RNN_WIDTH), D_MODEL ** -0.5),
        "rg_conv_w": nrm(ks[10], (N_ODD, RNN_CONV, RNN_WIDTH), RNN_CONV ** -0.5),
        "rg_conv_b": nrm(ks[11], (N_ODD, RNN_WIDTH), 0.01),
        "rg_w_a": nrm(ks[12], (N_ODD, RNN_BLOCKS, RNN_BLOCK, RNN_BLOCK), RNN_BLOCK ** -0.5),
        "rg_b_a": nrm(ks[13], (N_ODD, RNN_WIDTH), 0.01),
        "rg_w_x": nrm(ks[14], (N_ODD, RNN_BLOCKS, RNN_BLOCK, RNN_BLOCK), RNN_BLOCK ** -0.5),
        "rg_b_x": nrm(ks[15], (N_ODD, RNN_WIDTH), 0.01),
        "rg_lambda": jnp.log(a0) - jnp.log1p(-a0),
        "rg_w_out": nrm(ks[17], (N_ODD, RNN_WIDTH, D_MODEL), RNN_WIDTH ** -0.5),
        "ffn_w_in": nrm(ks[18], (DEPTH, D_MODEL, 2 * D_FF), D_MODEL ** -0.5),
        "ffn_conv_w": nrm(ks[19], (DEPTH, FFN_CONV, 2 * D_FF), FFN_CONV ** -0.5),
        "ffn_conv_b": nrm(ks[20], (DEPTH, 2 * D_FF), 0.01),
        "ffn_w_out": nrm(ks[21], (DEPTH, D_FF, D_MODEL), D_FF ** -0.5),
    }


def reference(x, norm_mix, norm_ffn, pa_w_in, pa_pool_w, pa_pool_scale, pa_q_gain, pa_k_gain,
              pa_w_out, rg_w_in, rg_conv_w, rg_conv_b, rg_w_a, rg_b_a, rg_w_x, rg_b_x, rg_lambda,
              rg_w_out, ffn_w_in, ffn_conv_w, ffn_conv_b, ffn_w_out):
    for l in range(DEPTH):
        j = l // 2
        h = rms_norm(x, norm_mix[l])
        if l % 2 == 0:
            x = x + pool_attn_mixer(h, pa_w_in[j], pa_pool_w[j], pa_pool_scale[j],
                                    pa_q_gain[j], pa_k_gain[j], pa_w_out[j])
        else:
            x = x + rglru_mixer(h, rg_w_in[j], rg_conv_w[j], rg_conv_b[j], rg_w_a[j], rg_b_a[j],
                                rg_w_x[j], rg_b_x[j], rg_lambda[j], rg_w_out[j])
        h = rms_norm(x, norm_ffn[l])
        x = x + conv_ffn(h, ffn_w_in[l], ffn_conv_w[l], ffn_conv_b[l], ffn_w_out[l])
    return x
```

```python
from contextlib import ExitStack
import numpy as np
import concourse.bass as bass
import concourse.mybir as mybir
from concourse.bass_utils import run_bass_kernel_spmd

F32 = mybir.dt.float32
BF16 = mybir.dt.bfloat16
AF = mybir.ActivationFunctionType
ALU = mybir.AluOpType
AX = mybir.AxisListType

D = 1024
NC_ = 8
DFF = 2816
NJ = 22
RW = 1280
NR = 10
TS = 512
EPS = 1e-6
NEG = -30000.0


class Res:
    __slots__ = ("w", "r")

    def __init__(self):
        self.w = None
        self.r = {}


class KB:
    def __init__(self, nc, es):
        self.nc = nc
        self.engs = {"pe": nc.tensor, "act": nc.scalar, "dve": nc.vector, "pool": nc.gpsimd, "sp": nc.sync}
        self.sem = {}
        self.cnt = {}
        for e in ("pe", "act", "dve", "pool"):
            self.sem[e] = es.enter_context(nc.semaphore("s_" + e))
            self.cnt[e] = 0
        self.waited = {e: {} for e in self.engs}
        self.rings = {}
        for q, n in (("sp", 24), ("pool", 12), ("act", 4)):
            self.rings[q] = {
                "sems": [es.enter_context(nc.semaphore(f"d_{q}{i}")) for i in range(n)],
                "val": [0] * n, "last": [None] * n, "i": 0,
            }
        self.ninst = 0

    def _wait(self, eng, tok):
        key, sem, val = tok
        if self.waited[eng].get(key, 0) >= val:
            return
        self.engs[eng].wait_ge(sem, val)
        self.waited[eng][key] = val

    def deps(self, eng, reads, writes):
        for r in reads:
            if r.w is not None:
                self._wait(eng, r.w)
        for w in writes:
            if w.w is not None and (w.w[0] != eng or eng != "pe"):
                self._wait(eng, w.w)
            for k, tok in w.r.items():
                if k != eng or eng != "pe":
                    self._wait(eng, tok)

    def op(self, eng, reads, writes, fn):
        self.deps(eng, reads, writes)
        ins = fn(self.engs[eng])
        self.cnt[eng] += 1
        tok = (eng, self.sem[eng], self.cnt[eng])
        ins.then_inc(self.sem[eng], 1)
        for r in reads:
            r.r[eng] = tok
        for w in writes:
            w.w = tok
            w.r = {}
        self.ninst += 1
        return tok

    def dma(self, q, out, in_, reads, writes):
        self.deps(q, reads, writes)
        ring = self.rings[q]
        i = ring["i"]
        ring["i"] = (i + 1) % len(ring["sems"])
        if ring["last"][i] is not None:
            self._wait(q, ring["last"][i])
        ring["val"][i] += 16
        key = f"dma_{q}{i}"
        tok = (key, ring["sems"][i], ring["val"][i])
        self.engs[q].dma_start(out=out, in_=in_).then_inc(ring["sems"][i], 16)
        ring["last"][i] = tok
        for r in reads:
            r.r[key] = tok
        for w in writes:
            w.w = tok
            w.r = {}
        self.ninst += 1
        return tok

    def barrier(self):
        toks = [(e, self.sem[e], self.cnt[e]) for e in ("pe", "act", "dve", "pool") if self.cnt[e]]
        for ring in self.rings.values():
            toks += [t for t in ring["last"] if t is not None]
        for e in self.engs:
            for t in toks:
                self._wait(e, t)

    def wait_all(self, eng, toks):
        for t in toks:
            if t is not None:
                self._wait(eng, t)


class Buf:
    def __init__(self, t, nparts=0):
        self.t = t
        self.res = Res()
        self.p = [Res() for _ in range(nparts)]


_UID = [0]


def alloc(es, nc, name, shape, dt, nparts=0):
    _UID[0] += 1
    return Buf(es.enter_context(nc.sbuf_tensor(f"{name}_{_UID[0]}", list(shape), dt)), nparts)


def palloc(es, nc, name, shape, dt=F32):
    _UID[0] += 1
    return Buf(es.enter_context(nc.psum_tensor(f"{name}_{_UID[0]}", list(shape), dt)))


def rmsnorm_tile(kb, X, H, G, ones, SQ, RS, ps_n, n=TS, hoff=0):
    SQl = SQ if isinstance(SQ, list) else [SQ]
    L = len(SQl)

    def square(c):
        sq = SQl[c % L]
        kb.op("act", [X.res], [sq.res],
              lambda e: e.activation(out=sq.t[:, 0:n], in_=X.t[:, c, 0:n], func=AF.Square))

    for c in range(min(L, NC_)):
        square(c)
    for c in range(NC_):
        sq = SQl[c % L]
        kb.op("pe", [sq.res, ones.res], [ps_n.res],
              lambda e, c=c, sq=sq: e.matmul(ps_n.t[:, 0:n], ones.t[:, :], sq.t[:, 0:n], start=(c == 0), stop=(c == NC_ - 1)))
        if c + L < NC_:
            square(c + L)
    kb.op("act", [ps_n.res, G["eps"].res], [RS.res],
          lambda e: e.activation(out=RS.t[:, 0:n], in_=ps_n.t[:, 0:n], func=AF.Sqrt, bias=G["eps"].t[:, 0:1], scale=1.0 / D))
    kb.op("dve", [RS.res], [RS.res], lambda e: e.reciprocal(out=RS.t[:, 0:n], in_=RS.t[:, 0:n]))
    for c in range(NC_):
        kb.op("dve", [X.res, RS.res, G["g"].res], [H.res],
              lambda e, c=c: e.scalar_tensor_tensor(out=H.t[:, c, hoff:hoff + n], in0=X.t[:, c, 0:n], scalar=G["g"].t[:, c:c + 1],
                                                    in1=RS.t[:, 0:n], op0=ALU.mult, op1=ALU.mult))


def emit_skewed(chains):
    if not chains:
        return
    S = 1 + max(max(c.keys()) for c in chains)
    n = len(chains)
    for step in range(n + S - 1):
        for st in range(S - 1, -1, -1):
            ci = step - st
            if 0 <= ci < n and st in chains[ci]:
                chains[ci][st]()


def ffn_pass(kb, P, l, x_src, x_dst, T, cst, scr):
    nc = kb.nc
    NT = T // TS
    w2s = scr["w2s"]
    with ExitStack() as es:
        NBLK = 8
        BW = DFF // NBLK
        W1 = alloc(es, nc, "f_w1", [128, NC_, 2 * DFF], BF16, NC_ * 2 * NBLK)
        W2S = [alloc(es, nc, f"f_w2s{i}", [128, NJ, 128], BF16) for i in range(2)]
        CW = alloc(es, nc, "f_cw", [128, 3, 2 * NJ], F32)
        CB = alloc(es, nc, "f_cb", [128, 2 * NJ], F32)
        Gg = alloc(es, nc, "f_g", [128, NC_], F32)
        TAIL = alloc(es, nc, "f_tail", [128, 2 * NJ, 2], F32)
        X = [alloc(es, nc, f"f_x{i}", [128, NC_, TS], F32) for i in range(2)]
        H = [alloc(es, nc, f"f_h{i}", [128, NC_, TS], BF16) for i in range(2)]
        ACTB = [alloc(es, nc, f"f_act{j}", [128, TS], BF16) for j in range(NJ)]
        ZS = [[alloc(es, nc, f"f_zs{i}_{u}", [128, TS + 2], F32, 1) for u in range(2)] for i in range(2)]
        CZ = [[alloc(es, nc, f"f_cz{i}_{u}", [128, TS], F32) for u in range(2)] for i in range(4)]
        GG = [alloc(es, nc, f"f_gg{i}", [128, TS], F32) for i in range(2)]
        SQ = [alloc(es, nc, f"f_sq{i}", [128, TS], BF16) for i in range(4)]
        RS = alloc(es, nc, "f_rs", [128, TS], F32)
        ps_z = [palloc(es, nc, f"f_psz{i}", [128, TS]) for i in range(4)]
        ps_o = [palloc(es, nc, f"f_pso{i}", [128, TS]) for i in range(2)]
        ps_n = palloc(es, nc, "f_psn", [128, TS])
        G = {"g": Gg, "eps": cst["eps"]}

        w_in = P["ffn_w_in"]
        for blk in range(NBLK):
            for ug in range(2):
                c0 = ug * DFF + blk * BW
                for c in range(NC_):
                    kb.dma("pool", W1.t[:, c, c0:c0 + BW], w_in[l, c * 128:(c + 1) * 128, c0:c0 + BW], [],
                           [W1.p[(ug * NBLK + blk) * NC_ + c]])
        r_w2 = [Res() for _ in range(NC_)]
        for m in range(NC_):
            kb.dma("pool", w2s[m], P["ffn_w_out"][l, :, m * 128:(m + 1) * 128].rearrange("(j p) n -> p j n", p=128), [], [r_w2[m]])
        kb.dma("sp", CW.t[:, :, :], P["ffn_cw"][l], [], [CW.res])
        kb.dma("sp", CB.t[:, :], P["ffn_cb"][l], [], [CB.res])
        kb.dma("sp", Gg.t[:, :], P["norm_ffn_t"][l], [], [Gg.res])
        kb.op("pool", [], [TAIL.res], lambda e: e.memset(TAIL.t[:, :, :], 0.0))

        chains = []
        kc = [0]

        def prologue(n):
            def f():
                t0 = n * TS
                Xn, Hn = X[n % 2], H[n % 2]
                kb.dma("sp", Xn.t[:, :, :], x_src[:, t0:t0 + TS].rearrange("(c p) t -> p c t", p=128), [], [Xn.res])
                rmsnorm_tile(kb, Xn, Hn, G, cst["ones"], SQ, RS, ps_n)
            return {0: f}

        def chain(n, j):
            k = kc[0]
            kc[0] += 1
            Hn = H[n % 2]
            pss = (ps_z[(k % 2) * 2], ps_z[(k % 2) * 2 + 1])
            zs2, cz2, gg = ZS[k % 2], CZ[k % 4], GG[k % 2]

            def s0():
                for ug in range(2):
                    jj = ug * NJ + j
                    ps = pss[ug]
                    blks = sorted({(j * 128) // BW, (j * 128 + 127) // BW})
                    for c in range(NC_):
                        kb.op("pe", [W1.p[(ug * NBLK + b) * NC_ + c] for b in blks] + [Hn.res], [ps.res],
                              lambda e, c=c, jj=jj, ps=ps: e.matmul(ps.t[:, :], W1.t[:, c, jj * 128:(jj + 1) * 128], Hn.t[:, c, :],
                                                                    start=(c == 0), stop=(c == NC_ - 1)))

            def s1():
                for ug in range(2):
                    jj = ug * NJ + j
                    zs = zs2[ug]
                    kb.op("pool", [TAIL.res], [zs.p[0]], lambda e, zs=zs, jj=jj: e.tensor_copy(out=zs.t[:, 0:2], in_=TAIL.t[:, jj, :]))
                for ug in range(2):
                    jj = ug * NJ + j
                    ps, zs, cz = pss[ug], zs2[ug], cz2[ug]
                    kb.op("act", [ps.res], [zs.res], lambda e, zs=zs, ps=ps: e.copy(out=zs.t[:, 2:TS + 2], in_=ps.t[:, :]))
                    kb.op("act", [zs.res, CW.res, CB.res], [cz.res],
                          lambda e, zs=zs, cz=cz, jj=jj: e.activation(out=cz.t[:, :], in_=zs.t[:, 2:TS + 2], func=AF.Identity,
                                                                      bias=CB.t[:, jj:jj + 1], scale=CW.t[:, 2, jj:jj + 1]))
                for ug in range(2):
                    jj = ug * NJ + j
                    zs = zs2[ug]
                    kb.op("pool", [zs.res], [TAIL.res], lambda e, zs=zs, jj=jj: e.tensor_copy(out=TAIL.t[:, jj, :], in_=zs.t[:, TS:TS + 2]))

            def s2():
                for ug in range(2):
                    jj = ug * NJ + j
                    zs, cz = zs2[ug], cz2[ug]
                    for kk in (1, 0):
                        kb.op("dve", [zs.res, zs.p[0], cz.res, CW.res], [cz.res],
                              lambda e, zs=zs, cz=cz, jj=jj, kk=kk: e.scalar_tensor_tensor(
                                  out=cz.t[:, :], in0=zs.t[:, kk:kk + TS], scalar=CW.t[:, kk, jj:jj + 1], in1=cz.t[:, :],
                                  op0=ALU.mult, op1=ALU.add))

            def s3():
                kb.op("act", [cz2[1].res], [gg.res], lambda e: e.activation(out=gg.t[:, :], in_=cz2[1].t[:, :], func=AF.Gelu_apprx_tanh))

            def s4():
                kb.op("dve", [gg.res, cz2[0].res], [ACTB[j].res],
                      lambda e: e.tensor_tensor(out=ACTB[j].t[:, :], in0=gg.t[:, :], in1=cz2[0].t[:, :], op=ALU.mult))

            return {0: s0, 1: s1, 2: s2, 3: s3, 4: s4}

        mcount = [0]

        def epilogue(n):
            def f():
                t0 = n * TS
                Xn = X[n % 2]
                for m in range(NC_):
                    wb = W2S[mcount[0] % 2]
                    mcount[0] += 1
                    kb.dma("sp", wb.t[:, :, :], w2s[m], [r_w2[m]], [wb.res])
                    ps = ps_o[m % 2]
                    for j in range(NJ):
                        kb.op("pe", [wb.res, ACTB[j].res], [ps.res],
                              lambda e, j=j, ps=ps, wb=wb: e.matmul(ps.t[:, :], wb.t[:, j, :], ACTB[j].t[:, :], start=(j == 0), stop=(j == NJ - 1)))
                    kb.op("dve", [ps.res, Xn.res], [Xn.res],
                          lambda e, m=m, ps=ps: e.tensor_tensor(out=Xn.t[:, m, :], in0=ps.t[:, :], in1=Xn.t[:, m, :], op=ALU.add))
                kb.dma("sp", x_dst[:, t0:t0 + TS].rearrange("(c p) t -> p c t", p=128), Xn.t[:, :, :], [Xn.res], [])
            return {4: f}

        for n in range(NT):
            chains.append(prologue(n))
            for j in range(NJ):
                chains.append(chain(n, j))
            chains.append(epilogue(n))
        emit_skewed(chains)


def rg_pass(kb, P, l, x_src, x_dst, T, cst):
    nc = kb.nc
    NT = T // TS
    jl = l // 2
    with ExitStack() as es:
        W1 = alloc(es, nc, "r_w1", [128, NC_, 2 * RW], BF16, NC_)
        W2 = alloc(es, nc, "r_w2", [128, NR, D], BF16, NR)
        WA = alloc(es, nc, "r_wa", [128, NR, 128], BF16)
        WX = alloc(es, nc, "r_wx", [128, NR, 128], BF16)
        CW = alloc(es, nc, "r_cw", [128, 4, NR], F32)
        VEC = alloc(es, nc, "r_vec", [128, 4, NR], F32)
        C8 = alloc(es, nc, "r_c8", [128, NR], F32)
        C16 = alloc(es, nc, "r_c16", [128, NR], F32)
        Gg = alloc(es, nc, "r_g", [128, NC_], F32)
        TAIL = alloc(es, nc, "r_tail", [128, NR, 3], F32)
        STATE = alloc(es, nc, "r_state", [128, NR], F32)
        X = [alloc(es, nc, f"r_x{i}", [128, NC_, TS], F32) for i in range(2)]
        H = [alloc(es, nc, f"r_h{i}", [128, NC_, TS], BF16) for i in range(2)]
        Y = [[alloc(es, nc, f"r_y{i}_{j}", [128, TS], BF16) for j in range(NR)] for i in range(2)]

        def mk(nm, cnt, dt=F32, w=TS):
            return [alloc(es, nc, f"r_{nm}{i}", [128, w], dt, 1) for i in range(cnt)]
        GGs, XCs, XSs, XCBs = mk("gg", 5), mk("xc", 5), mk("xs", 2, w=TS + 3), mk("xcb", 2, BF16)
        Rs, Is, As, Ss, Bs, HSs = mk("r", 2), mk("i", 2), mk("a", 2), mk("s", 2), mk("b", 2), mk("hs", 2)
        SQ = [alloc(es, nc, f"r_sq{i}", [128, TS], BF16) for i in range(8)]
        RS = alloc(es, nc, "r_rs", [128, TS], F32)
        ps_gx = [palloc(es, nc, f"r_psgx{i}", [128, TS]) for i in range(4)]
        ps_ri = [palloc(es, nc, f"r_psri{i}", [128, TS]) for i in range(2)]
        ps_o = palloc(es, nc, "r_pso", [128, TS])
        ps_n = palloc(es, nc, "r_psn", [128, TS])
        G = {"g": Gg, "eps": cst["eps"]}

        for c in range(NC_):
            kb.dma("pool", W1.t[:, c, :], P["rg_w_in"][jl, c * 128:(c + 1) * 128, :], [], [W1.p[c]])
        for j in range(NR):
            kb.dma("pool", W2.t[:, j, :], P["rg_w_out"][jl, j * 128:(j + 1) * 128, :], [], [W2.p[j]])
        kb.dma("pool", WA.t[:, :, :], P["rg_w_a"][jl].rearrange("n c d -> c n d"), [], [WA.res])
        kb.dma("pool", WX.t[:, :, :], P["rg_w_x"][jl].rearrange("n c d -> c n d"), [], [WX.res])
        kb.dma("sp", CW.t[:, :, :], P["rg_cw"][jl], [], [CW.res])
        kb.dma("sp", VEC.t[:, :, :], P["rg_vec"][jl], [], [VEC.res])
        kb.dma("sp", Gg.t[:, :], P["norm_mix_t"][l], [], [Gg.res])
        kb.op("pool", [], [TAIL.res], lambda e: e.memset(TAIL.t[:, :, :], 0.0))
        kb.op("pool", [], [STATE.res], lambda e: e.memset(STATE.t[:, :], 0.0))
        kb.op("act", [VEC.res], [C8.res], lambda e: e.activation(out=C8.t[:, :], in_=VEC.t[:, 3, :], func=AF.Exp, scale=-1.0))
        kb.op("act", [C8.res, cst["one"].res], [C8.res],
              lambda e: e.activation(out=C8.t[:, :], in_=C8.t[:, :], func=AF.Ln, bias=cst["one"].t[:, 0:1], scale=1.0))
        kb.op("dve", [C8.res], [C16.res], lambda e: e.tensor_scalar(out=C16.t[:, :], in0=C8.t[:, :], scalar1=-16.0, scalar2=None, op0=ALU.mult))
        kb.op("dve", [C8.res], [C8.res], lambda e: e.tensor_scalar(out=C8.t[:, :], in0=C8.t[:, :], scalar1=-8.0, scalar2=None, op0=ALU.mult))

        kc = [0]

        def prologue(n):
            def f():
                t0 = n * TS
                Xn, Hn = X[n % 2], H[n % 2]
                kb.dma("sp", Xn.t[:, :, :], x_src[:, t0:t0 + TS].rearrange("(c p) t -> p c t", p=128), [], [Xn.res])
                rmsnorm_tile(kb, Xn, Hn, G, cst["ones"], SQ, RS, ps_n)
            return {0: f}

        def chain(n, j):
            k = kc[0]
            kc[0] += 1
            Hn = H[n % 2]
            pg, px = ps_gx[(k % 2) * 2], ps_gx[(k % 2) * 2 + 1]
            gg, xc, xs, xcb = GGs[k % 5], XCs[k % 5], XSs[k % 2], XCBs[k % 2]
            r, ii, a, sq_, b, hs = Rs[k % 2], Is[k % 2], As[k % 2], Ss[k % 2], Bs[k % 2], HSs[k % 2]
            Yj = Y[n % 2][j]

            def s0():
                for (ps, off) in ((pg, j * 128), (px, RW + j * 128)):
                    for c in range(NC_):
                        kb.op("pe", [W1.p[c], Hn.res], [ps.res],
                              lambda e, c=c, ps=ps, off=off: e.matmul(ps.t[:, :], W1.t[:, c, off:off + 128], Hn.t[:, c, :],
                                                                      start=(c == 0), stop=(c == NC_ - 1)))

            def s1():
                kb.op("pool", [TAIL.res], [xs.p[0]], lambda e: e.tensor_copy(out=xs.t[:, 0:3], in_=TAIL.t[:, j, :]))
                kb.op("dve", [px.res], [xs.res], lambda e: e.tensor_copy(out=xs.t[:, 3:TS + 3], in_=px.t[:, :]))
                kb.op("pool", [xs.res], [TAIL.res], lambda e: e.tensor_copy(out=TAIL.t[:, j, :], in_=xs.t[:, TS:TS + 3]))
                kb.op("dve", [xs.res, CW.res, VEC.res], [xc.res],
                      lambda e: e.tensor_scalar(out=xc.t[:, :], in0=xs.t[:, 3:TS + 3], scalar1=CW.t[:, 3, j:j + 1],
                                                scalar2=VEC.t[:, 0, j:j + 1], op0=ALU.mult, op1=ALU.add))
                kb.op("act", [pg.res], [gg.res], lambda e: e.activation(out=gg.t[:, :], in_=pg.t[:, :], func=AF.Gelu_apprx_tanh))

            def s2():
                for kk in (2, 1, 0):
                    kb.op("dve", [xs.res, xs.p[0], xc.res, CW.res], [xc.res],
                          lambda e, kk=kk: e.scalar_tensor_tensor(
                              out=xc.t[:, :], in0=xs.t[:, kk:kk + TS], scalar=CW.t[:, kk, j:j + 1], in1=xc.t[:, :], op0=ALU.mult, op1=ALU.add))
                kb.op("pool", [xc.res], [xcb.res], lambda e: e.tensor_copy(out=xcb.t[:, :], in_=xc.t[:, :]))

            def s3():
                kb.op("pe", [WA.res, xcb.res], [ps_ri[0].res],
                      lambda e: e.matmul(ps_ri[0].t[:, :], WA.t[:, j, :], xcb.t[:, :], start=True, stop=True))
                kb.op("pe", [WX.res, xcb.res], [ps_ri[1].res],
                      lambda e: e.matmul(ps_ri[1].t[:, :], WX.t[:, j, :], xcb.t[:, :], start=True, stop=True))

            def s4():
                kb.op("act", [ps_ri[0].res, VEC.res], [r.res],
                      lambda e: e.activation(out=r.t[:, :], in_=ps_ri[0].t[:, :], func=AF.Sigmoid, bias=VEC.t[:, 1, j:j + 1], scale=1.0))
                kb.op("act", [ps_ri[1].res, VEC.res], [ii.res],
                      lambda e: e.activation(out=ii.t[:, :], in_=ps_ri[1].t[:, :], func=AF.Sigmoid, bias=VEC.t[:, 2, j:j + 1], scale=1.0))
                kb.op("act", [r.res, C8.res], [a.res],
                      lambda e: e.activation(out=a.t[:, :], in_=r.t[:, :], func=AF.Exp, scale=C8.t[:, j:j + 1]))
                kb.op("act", [r.res, C16.res], [sq_.res],
                      lambda e: e.activation(out=sq_.t[:, :], in_=r.t[:, :], func=AF.Exp, scale=C16.t[:, j:j + 1]))
                kb.op("act", [sq_.res, cst["one"].res], [sq_.res],
                      lambda e: e.activation(out=sq_.t[:, :], in_=sq_.t[:, :], func=AF.Sqrt, bias=cst["one"].t[:, 0:1], scale=-1.0))

            def s5():
                kb.op("dve", [ii.res, xc.res], [b.res], lambda e: e.tensor_tensor(out=b.t[:, :], in0=ii.t[:, :], in1=xc.t[:, :], op=ALU.mult))
                kb.op("dve", [b.res, sq_.res], [b.res], lambda e: e.tensor_tensor(out=b.t[:, :], in0=b.t[:, :], in1=sq_.t[:, :], op=ALU.mult))
                kb.op("dve", [a.res, b.res, STATE.res], [hs.res],
                      lambda e: e.tensor_tensor_scan(out=hs.t[:, :], data0=a.t[:, :], data1=b.t[:, :],
                                                     initial=STATE.t[:, j:j + 1], op0=ALU.mult, op1=ALU.add))
                kb.op("pool", [hs.res], [STATE.res], lambda e: e.tensor_copy(out=STATE.t[:, j:j + 1], in_=hs.t[:, TS - 1:TS]))
                kb.op("dve", [hs.res, gg.res], [Yj.res], lambda e: e.tensor_tensor(out=Yj.t[:, :], in0=gg.t[:, :], in1=hs.t[:, :], op=ALU.mult))

            return {0: s0, 1: s1, 2: s2, 3: s3, 4: s4, 5: s5}

        def epilogue(n):
            def f():
                t0 = n * TS
                Xn = X[n % 2]
                for m in range(NC_):
                    for j in range(NR):
                        Yj = Y[n % 2][j]
                        kb.op("pe", [W2.p[j], Yj.res], [ps_o.res],
                              lambda e, m=m, j=j, Yj=Yj: e.matmul(ps_o.t[:, :], W2.t[:, j, m * 128:(m + 1) * 128], Yj.t[:, :],
                                                                  start=(j == 0), stop=(j == NR - 1)))
                    kb.op("dve", [ps_o.res, Xn.res], [Xn.res],
                          lambda e, m=m: e.tensor_tensor(out=Xn.t[:, m, :], in0=ps_o.t[:, :], in1=Xn.t[:, m, :], op=ALU.add))
                kb.dma("sp", x_dst[:, t0:t0 + TS].rearrange("(c p) t -> p c t", p=128), Xn.t[:, :, :], [Xn.res], [])
            return {5: f}

        chains = []
        for n in range(NT):
            chains.append(prologue(n))
            for j in range(NR):
                chains.append(chain(n, j))
            chains.append(epilogue(n))
        emit_skewed(chains)


def pa_pass(kb, P, l, x_src, x_dst, T, cst, scr):
    nc = kb.nc
    NT = T // TS
    jl = l // 2
    qd, kd, vd, pad = scr["qd"], scr["kd"], scr["vd"], scr["pad"]
    r_q = [[] for _ in range(NT)]
    r_k = [[] for _ in range(NT)]
    r_v = [[] for _ in range(NT)]
    r_p = [[] for _ in range(NT)]
    aux = scr["aux_res"]

    def nr(lst):
        r = Res()
        lst.append(r)
        return [r]

    def cat(lsts):
        out = []
        for x in lsts:
            out += x
        return out
    with ExitStack() as es:
        WIN = alloc(es, nc, "p_win", [128, NC_, 2048], BF16, NC_)
        PW = alloc(es, nc, "p_pw", [128, 4, 128], BF16)
        VEC = alloc(es, nc, "p_vec", [128, 6], F32)
        QG8 = alloc(es, nc, "p_qg8", [128, 1], F32)
        Gg = alloc(es, nc, "p_g", [128, NC_], F32)
        RC = alloc(es, nc, "p_rc", [128, 4, TS], F32)
        PBI = alloc(es, nc, "p_pbi", [128, TS], F32)
        PB3 = alloc(es, nc, "p_pb3", [128, TS], F32)
        PTAIL = alloc(es, nc, "p_ptail", [128, 4, 16], F32)
        KM = alloc(es, nc, "p_km", [128, 4, 16], F32)
        X = alloc(es, nc, "p_x", [128, NC_, TS], F32)
        H = alloc(es, nc, "p_h", [128, NC_, TS], BF16)
        US = [alloc(es, nc, f"p_us{i}", [128, TS + 16], F32) for i in range(2)]
        SA = alloc(es, nc, "p_sa", [128, TS + 16], F32)
        SB = alloc(es, nc, "p_sb", [128, TS + 16], F32)
        PBF = [alloc(es, nc, f"p_pbf{i}", [128, TS], BF16) for i in range(2)]
        PAo = [alloc(es, nc, f"p_pao{i}", [128, TS], BF16) for i in range(2)]
        SQh = [alloc(es, nc, f"p_sqh{i}", [128, TS], BF16) for i in range(2)]
        RSh = [alloc(es, nc, f"p_rsh{i}", [128, TS], F32) for i in range(2)]
        NF = [alloc(es, nc, f"p_nf{i}", [128, TS], F32) for i in range(2)]
        NB = [alloc(es, nc, f"p_nb{i}", [128, TS], BF16) for i in range(2)]
        KS = alloc(es, nc, "p_ks", [128, 2], F32)
        GP = alloc(es, nc, "p_gp", [128, TS], F32)
        M8 = alloc(es, nc, "p_m8", [128, 32, 8], F32)
        MBF = alloc(es, nc, "p_mbf", [128, TS], F32)
        MBB = alloc(es, nc, "p_mbb", [128, TS], BF16)
        MT = [alloc(es, nc, f"p_mt{i}", [16, TS], BF16) for i in range(2)]
        VT = [alloc(es, nc, f"p_vt{i}", [128, TS], BF16) for i in range(2)]
        SQ = [alloc(es, nc, f"p_sq{i}", [128, TS], BF16) for i in range(8)]
        RS = alloc(es, nc, "p_rs", [128, TS], F32)
        ps_n = palloc(es, nc, "p_psn", [128, TS])
        ps_a = [palloc(es, nc, f"p_psa{i}", [128, TS]) for i in range(2)]
        ps_ms = palloc(es, nc, "p_psms", [128, TS])
        ps_pw = palloc(es, nc, "p_pspw", [128, TS])
        ps_g = palloc(es, nc, "p_psg", [128, TS])
        ps_t = palloc(es, nc, "p_pst", [16, TS], BF16)
        G = {"g": Gg, "eps": cst["eps"]}

        for c in range(NC_):
            kb.dma("pool", WIN.t[:, c, :], P["pa_w_in"][jl, c * 128:(c + 1) * 128, :], [], [WIN.p[c]])
        kb.dma("pool", PW.t[:, :, :], P["pa_pool_w"][jl].rearrange("g c d -> c g d"), [], [PW.res])
        kb.dma("sp", VEC.t[:, :], P["pa_vec"][jl], [], [VEC.res])
        kb.dma("sp", Gg.t[:, :], P["norm_mix_t"][l], [], [Gg.res])
        kb.dma("sp", RC.t[:, :, :], P["c_rc"], [], [RC.res])
        kb.op("dve", [VEC.res], [QG8.res], lambda e: e.tensor_scalar(out=QG8.t[:, :], in0=VEC.t[:, 4:5], scalar1=0.125, scalar2=None, op0=ALU.mult))
        kb.op("pool", [], [PTAIL.res], lambda e: e.memset(PTAIL.t[:, :, :], 0.0))
        kb.op("pool", [], [KM.res], lambda e: e.memset(KM.t[:, :, :], 0.0))

        def head_norm(ps, gcol, i):
            kb.op("act", [ps.res], [SQh[i].res], lambda e: e.activation(out=SQh[i].t[:, :], in_=ps.t[:, :], func=AF.Square))
            kb.op("pe", [SQh[i].res, cst["bones"].res], [ps_ms.res],
                  lambda e: e.matmul(ps_ms.t[:, :], cst["bones"].t[:, :], SQh[i].t[:, :], start=True, stop=True))
            kb.op("act", [ps_ms.res, cst["eps"].res], [RSh[i].res],
                  lambda e: e.activation(out=RSh[i].t[:, :], in_=ps_ms.t[:, :], func=AF.Sqrt, bias=cst["eps"].t[:, 0:1], scale=1.0 / 64))
            kb.op("dve", [RSh[i].res], [RSh[i].res], lambda e: e.reciprocal(out=RSh[i].t[:, :], in_=RSh[i].t[:, :]))
            kb.op("dve", [ps.res, RSh[i].res, VEC.res], [NF[i].res],
                  lambda e: e.scalar_tensor_tensor(out=NF[i].t[:, :], in0=ps.t[:, :], scalar=VEC.t[:, gcol:gcol + 1], in1=RSh[i].t[:, :],
                                                   op0=ALU.mult, op1=ALU.mult))

        def proj(ps, off):
            for c in range(NC_):
                kb.op("pe", [WIN.p[c], H.res], [ps.res],
                      lambda e, c=c: e.matmul(ps.t[:, :], WIN.t[:, c, off:off + 128], H.t[:, c, :], start=(c == 0), stop=(c == NC_ - 1)))

        ia = 0
        for n in range(NT):
            t0 = n * TS
            kb.dma("sp", X.t[:, :, :], x_src[:, t0:t0 + TS].rearrange("(c p) t -> p c t", p=128), [], [X.res])
            kb.dma("sp", PBI.t[:, :], P["c_pbinf"][n], [], [PBI.res])
            kb.dma("sp", PB3.t[:, :], P["c_pb30k"][n], [], [PB3.res])
            rmsnorm_tile(kb, X, H, G, cst["ones"], SQ, RS, ps_n)
            for g in range(4):
                ps = ps_a[ia % 2]; ia += 1
                proj(ps, g * 128)
                us = US[g % 2]
                kb.op("pool", [PTAIL.res], [us.res], lambda e, us=us, g=g: e.tensor_copy(out=us.t[:, 0:16], in_=PTAIL.t[:, g, :]))
                kb.op("act", [ps.res], [us.res], lambda e, us=us, ps=ps: e.copy(out=us.t[:, 16:TS + 16], in_=ps.t[:, :]))
                kb.op("pool", [us.res], [PTAIL.res], lambda e, us=us, g=g: e.tensor_copy(out=PTAIL.t[:, g, :], in_=us.t[:, TS:TS + 16]))
                src_b, step, L = us, 1, TS + 16
                bufs = [SA, SB]
                bi = 0
                lo = 0
                for _ in range(g + 1):
                    dst_b = bufs[bi]; bi ^= 1
                    nlo = lo + step
                    kb.op("dve", [src_b.res], [dst_b.res],
                          lambda e, src_b=src_b, dst_b=dst_b, nlo=nlo, step=step: e.tensor_tensor(
                              out=dst_b.t[:, nlo:L], in0=src_b.t[:, nlo:L], in1=src_b.t[:, nlo - step:L - step], op=ALU.add))
                    src_b, lo, step = dst_b, nlo, step * 2
                w = 2 ** (g + 1)
                pb = PBF[g % 2]
                if n == 0:
                    kb.op("dve", [src_b.res, RC.res], [src_b.res],
                          lambda e, src_b=src_b, g=g: e.tensor_tensor(out=src_b.t[:, 16:L], in0=src_b.t[:, 16:L], in1=RC.t[:, g, :], op=ALU.mult))
                    kb.op("dve", [src_b.res, us.res], [pb.res],
                          lambda e, src_b=src_b, us=us, pb=pb: e.tensor_tensor(out=pb.t[:, :], in0=src_b.t[:, 16:L], in1=us.t[:, 16:L], op=ALU.subtract))
                else:
                    kb.op("dve", [src_b.res, us.res], [pb.res],
                          lambda e, src_b=src_b, us=us, pb=pb, w=w: e.scalar_tensor_tensor(
                              out=pb.t[:, :], in0=src_b.t[:, 16:L], scalar=1.0 / w, in1=us.t[:, 16:L], op0=ALU.mult, op1=ALU.subtract))
                kb.op("pe", [PW.res, pb.res], [ps_pw.res],
                      lambda e, g=g, pb=pb: e.matmul(ps_pw.t[:, :], PW.t[:, g, :], pb.t[:, :], start=True, stop=True))
                po = PAo[g % 2]
                kb.op("act", [ps_pw.res, VEC.res], [po.res],
                      lambda e, g=g, po=po: e.activation(out=po.t[:, :], in_=ps_pw.t[:, :], func=AF.Identity, bias=0.0, scale=VEC.t[:, g:g + 1]))
                kb.dma("sp", pad[g * 128:(g + 1) * 128, t0:t0 + TS], po.t[:, :], [po.res], nr(r_p[n]))
            for pr in range(4):
                ps = ps_a[ia % 2]; ia += 1
                proj(ps, 1024 + pr * 128)
                i = pr % 2
                head_norm(ps, 5, i)
                kb.op("pool", [NF[i].res], [NB[i].res], lambda e, i=i: e.tensor_copy(out=NB[i].t[:, :], in_=NF[i].t[:, :]))
                for hh in range(2):
                    kb.dma("sp", kd[2 * pr + hh, 0:64, t0:t0 + TS], NB[i].t[hh * 64:(hh + 1) * 64, :], [NB[i].res], nr(r_k[n]))
                kb.op("dve", [NF[i].res], [KS.res],
                      lambda e, i=i: e.tensor_reduce(out=KS.t[:, :], in_=NF[i].t[:, :].rearrange("p (b t) -> p b t", b=2), axis=AX.X, op=ALU.add))
                kb.op("dve", [KS.res], [KM.res],
                      lambda e, pr=pr, n=n: e.tensor_scalar(out=KM.t[:, pr, 2 * n:2 * n + 2], in0=KS.t[:, :], scalar1=1.0 / 256, scalar2=None, op0=ALU.mult))
            for pr in range(4):
                ps = ps_a[ia % 2]; ia += 1
                proj(ps, 512 + pr * 128)
                i = pr % 2
                head_norm(ps, 4, i)
                kb.op("act", [NF[i].res], [NB[i].res], lambda e, i=i: e.mul(out=NB[i].t[:, :], in_=NF[i].t[:, :], mul=0.125))
                for hh in range(2):
                    kb.dma("sp", qd[2 * pr + hh, 0:64, t0:t0 + TS], NB[i].t[hh * 64:(hh + 1) * 64, :], [NB[i].res], nr(r_q[n]))
                for sub in range(4):
                    for hh in range(2):
                        hd = 2 * pr + hh
                        o = (sub * 8 + hd) * 16
                        kb.op("pe", [NF[i].res, KM.res], [ps_g.res],
                              lambda e, i=i, sub=sub, hh=hh, pr=pr, o=o: e.matmul(
                                  ps_g.t[:, o:o + 16], NF[i].t[hh * 64:(hh + 1) * 64, sub * 128:(sub + 1) * 128],
                                  KM.t[hh * 64:(hh + 1) * 64, pr, :], start=True, stop=True))
            kb.op("dve", [ps_g.res, PBI.res], [GP.res], lambda e: e.tensor_tensor(out=GP.t[:, :], in0=ps_g.t[:, :], in1=PBI.t[:, :], op=ALU.add))
            for sh in range(32):
                kb.op("dve", [GP.res], [M8.res], lambda e, sh=sh: e.max(out=M8.t[:, sh, :], in_=GP.t[:, sh * 16:(sh + 1) * 16]))
            for sh in range(32):
                kb.op("dve", [GP.res, M8.res], [MBF.res],
                      lambda e, sh=sh: e.tensor_scalar(out=MBF.t[:, sh * 16:(sh + 1) * 16], in0=GP.t[:, sh * 16:(sh + 1) * 16],
                                                       scalar1=M8.t[:, sh, 3:4], scalar2=-NEG, op0=ALU.is_ge, op1=ALU.mult))
            kb.op("dve", [MBF.res, PB3.res], [MBB.res], lambda e: e.tensor_tensor(out=MBB.t[:, :], in0=MBF.t[:, :], in1=PB3.t[:, :], op=ALU.add))
            for hd in range(8):
                for sub in range(4):
                    o = (sub * 8 + hd) * 16
                    kb.op("pe", [MBB.res, cst["ident"].res], [ps_t.res],
                          lambda e, sub=sub, o=o: e.transpose(ps_t.t[:, sub * 128:(sub + 1) * 128], MBB.t[:, o:o + 16], cst["ident"].t[:, :]))
                mt = MT[hd % 2]
                kb.op("act", [ps_t.res], [mt.res], lambda e, mt=mt: e.copy(out=mt.t[:, :], in_=ps_t.t[:, :]))
                kb.dma("sp", qd[hd, 64:80, t0:t0 + TS], mt.t[:, :], [mt.res], nr(r_q[n]))
            for sub in range(4):
                ps = ps_a[ia % 2]; ia += 1
                for c in range(NC_):
                    kb.op("pe", [WIN.p[c], H.res], [ps.res],
                          lambda e, c=c, sub=sub, ps=ps: e.matmul(ps.t[:, :], H.t[:, c, sub * 128:(sub + 1) * 128], WIN.t[:, c, 1536:2048],
                                                                  start=(c == 0), stop=(c == NC_ - 1)))
                vt = VT[sub % 2]
                kb.op("act", [ps.res], [vt.res], lambda e, vt=vt, ps=ps: e.copy(out=vt.t[:, :], in_=ps.t[:, :]))
                kb.dma("sp", vd[t0 + sub * 128:t0 + (sub + 1) * 128, :], vt.t[:, :], [vt.res], nr(r_v[n]))
    kb.barrier()
    with ExitStack() as es:
        WOP = alloc(es, nc, "a_wop", [128, 4, D], BF16, 4)
        WOA = alloc(es, nc, "a_woa", [64, 8, D], BF16, 8)
        CMB = alloc(es, nc, "a_cmb", [128, 4, TS], BF16)
        X = [alloc(es, nc, f"a_x{i}", [128, NC_, TS], F32) for i in range(2)]
        PAg = [alloc(es, nc, f"a_pag{i}", [128, 4, TS], BF16) for i in range(2)]
        QP = [alloc(es, nc, f"a_qp{i}", [84, TS], BF16) for i in range(2)]
        KP = [alloc(es, nc, f"a_kp{i}", [84, T], BF16) for i in range(2)]
        VS = [alloc(es, nc, f"a_vs{i}", [128, T // 128, 65], BF16) for i in range(2)]
        PT = [alloc(es, nc, f"a_pt{i}", [128, TS], BF16) for i in range(4)]
        RD = alloc(es, nc, "a_rd", [65, TS], F32)
        BC = alloc(es, nc, "a_bc", [64, TS], F32)
        ATT = [[alloc(es, nc, f"a_att{i}_{h}", [64, TS], BF16) for h in range(8)] for i in range(2)]
        ps_s = [palloc(es, nc, f"a_pss{i}", [128, TS]) for i in range(3)]
        ps_acc = [palloc(es, nc, f"a_psacc{i}", [65, TS]) for i in range(2)]
        ps_b = palloc(es, nc, "a_psb", [64, TS])
        ps_o = [palloc(es, nc, f"a_pso{i}", [128, TS]) for i in range(2)]

        for g in range(4):
            kb.dma("pool", WOP.t[:, g, :], P["pa_w_out"][jl, g * 128:(g + 1) * 128, :], [], [WOP.p[g]])
        for h in range(8):
            kb.dma("pool", WOA.t[:, h, :], P["pa_w_out"][jl, 512 + h * 64:512 + (h + 1) * 64, :], [], [WOA.p[h]])
        kb.dma("pool", CMB.t[:, :, :], P["c_cmb"].rearrange("i p c -> p i c"), [], [CMB.res])
        for i in range(2):
            kb.op("pool", [], [VS[i].res], lambda e, i=i: e.memset(VS[i].t[:, :, 64:65], 1.0))

        def tile_prologue(n):
            def f():
                t0 = n * TS
                Xn, Pn = X[n % 2], PAg[n % 2]
                kb.dma("sp", Xn.t[:, :, :], x_src[:, t0:t0 + TS].rearrange("(c p) t -> p c t", p=128), [], [Xn.res])
                kb.dma("sp", Pn.t[:, :, :], pad[:, t0:t0 + TS].rearrange("(g p) t -> p g t", p=128), r_p[n], [Pn.res])
            return {0: f}

        def head_prologue(n, h):
            def f():
                t0 = n * TS
                nk = 4 * n + 4
                b = (n * 8 + h) % 2
                kb.dma("sp", QP[b].t[:, :], qd[h, :, t0:t0 + TS], r_q[n] + aux, [QP[b].res])
                kb.dma("sp", KP[b].t[:, 0:nk * 128], kd[h, :, 0:nk * 128], cat(r_k[0:n + 1]) + aux, [KP[b].res])
                kb.dma("sp", VS[b].t[:, 0:nk, 0:64], vd[0:nk * 128, h * 64:(h + 1) * 64].rearrange("(k p) d -> p k d", p=128),
                       cat(r_v[0:n + 1]), [VS[b].res])
            return {3: f}

        kcnt = [0]

        def kt_chain(n, h, kt):
            k = kcnt[0]
            kcnt[0] += 1
            nk = 4 * n + 4
            b = (n * 8 + h) % 2
            acc = ps_acc[(n * 8 + h) % 2]
            ps, pt = ps_s[k % 3], PT[k % 4]
            diag = kt >= 4 * n

            def s0():
                kb.op("pe", [KP[b].res, QP[b].res], [ps.res],
                      lambda e: e.matmul(ps.t[:, :], KP[b].t[:, kt * 128:(kt + 1) * 128], QP[b].t[:, :], start=True, stop=not diag))
                if diag:
                    kb.op("pe", [CMB.res, cst["ident"].res], [ps.res],
                          lambda e: e.matmul(ps.t[:, :], cst["ident"].t[:, :], CMB.t[:, kt - 4 * n, :], start=False, stop=True))

            def s1():
                kb.op("act", [ps.res], [pt.res], lambda e: e.activation(out=pt.t[:, :], in_=ps.t[:, :], func=AF.Exp))

            def s3():
                kb.op("pe", [VS[b].res, pt.res], [acc.res],
                      lambda e: e.matmul(acc.t[:, :], VS[b].t[:, kt, :], pt.t[:, :], start=(kt == 0), stop=(kt == nk - 1)))

            return {0: s0, 1: s1, 3: s3}

        def head_epilogue(n, h):
            acc = ps_acc[(n * 8 + h) % 2]
            att = ATT[n % 2][h]

            def f1():
                kb.op("act", [acc.res], [RD.res], lambda e: e.copy(out=RD.t[64:65, :], in_=acc.t[64:65, :]))
                kb.op("pe", [RD.res, cst["onesf"].res], [ps_b.res],
                      lambda e: e.matmul(ps_b.t[:, :], cst["onesf"].t[64:65, 0:64], RD.t[64:65, :], start=True, stop=True))

            def f2():
                kb.op("dve", [ps_b.res], [BC.res], lambda e: e.reciprocal(out=BC.t[:, :], in_=ps_b.t[:, :]))
                kb.op("dve", [acc.res, BC.res], [att.res],
                      lambda e: e.tensor_tensor(out=att.t[:, :], in0=acc.t[0:64, :], in1=BC.t[:, :], op=ALU.mult))
            return {4: f1, 5: f2}

        def tile_epilogue(n):
            def f():
                t0 = n * TS
                Xn, Pn = X[n % 2], PAg[n % 2]
                for m in range(NC_):
                    po = ps_o[m % 2]
                    for g in range(4):
                        kb.op("pe", [WOP.p[g], Pn.res], [po.res],
                              lambda e, m=m, g=g, po=po: e.matmul(po.t[:, :], WOP.t[:, g, m * 128:(m + 1) * 128], Pn.t[:, g, :], start=(g == 0), stop=False))
                    for h in range(8):
                        att = ATT[n % 2][h]
                        kb.op("pe", [WOA.p[h], att.res], [po.res],
                              lambda e, m=m, h=h, att=att, po=po: e.matmul(po.t[:, :], WOA.t[:, h, m * 128:(m + 1) * 128], att.t[:, :], start=False, stop=(h == 7)))
                    kb.op("dve", [po.res, Xn.res], [Xn.res],
                          lambda e, m=m, po=po: e.tensor_tensor(out=Xn.t[:, m, :], in0=po.t[:, :], in1=Xn.t[:, m, :], op=ALU.add))
                kb.dma("sp", x_dst[:, t0:t0 + TS].rearrange("(c p) t -> p c t", p=128), Xn.t[:, :, :], [Xn.res], [])
            return {7: f}

        chains = []
        heads = [(n, h) for n in range(NT) for h in range(8)]
        chains.append(head_prologue(*heads[0]))
        for idx, (n, h) in enumerate(heads):
            if h == 0:
                chains.append(tile_prologue(n))
            if idx + 1 < len(heads):
                chains.append(head_prologue(*heads[idx + 1]))
            for kt in range(4 * n + 4):
                chains.append(kt_chain(n, h, kt))
            chains.append(head_epilogue(n, h))
            if h == 7:
                chains.append(tile_epilogue(n))
        emit_skewed(chains)


def make_consts(T):
    NT = T // TS
    t = np.arange(T)
    slopes = 2.0 ** (-(np.arange(8) + 1.0))
    a, b = (t // 64).astype(np.float32), (t % 64).astype(np.float32)
    kaux = np.zeros((8, 20, T), np.float32)
    qaux = np.zeros((8, 4, T), np.float32)
    for h in range(8):
        kaux[h, (t // 256), t] = 1.0
        kaux[h, 16] = slopes[h] * 64 * a
        kaux[h, 17] = slopes[h] * b
        kaux[h, 18] = 1.0
        kaux[h, 19] = 1.0
        qaux[h, 0] = 1.0
        qaux[h, 1] = 1.0
        qaux[h, 2] = -slopes[h] * 64 * a
        qaux[h, 3] = -slopes[h] * b
    pbinf = np.zeros((NT, 128, 4, 8, 16), np.float32)
    pb30k = np.zeros((NT, 128, 4, 8, 16), np.float32)
    blk = np.arange(16)
    for n in range(NT):
        for sub in range(4):
            bq = 2 * n + sub // 2
            pbinf[n, :, sub, :, :] = np.where(blk < bq, 0.0, np.where(blk == bq, 1e30, -1e30))
            pb30k[n, :, sub, :, :] = np.where(blk <= bq, NEG, 2 * NEG)
    rc = np.zeros((128, 4, TS), np.float32)
    for g in range(4):
        rc[:, g, :] = 1.0 / np.minimum(np.arange(TS) + 1.0, 2.0 ** (g + 1))
    cmb = np.zeros((4, 128, TS), np.float32)
    pp = np.arange(128)[:, None]
    cc = np.arange(TS)[None, :]
    for i in range(4):
        cmb[i] = np.where(cc >= i * 128 + pp, 0.0, NEG)
    bones = np.zeros((128, 128), np.float32)
    bones[:64, :64] = 1.0
    bones[64:, 64:] = 1.0
    return {"c_kaux": kaux, "c_qaux": qaux, "c_pbinf": pbinf.reshape(NT, 128, TS), "c_pb30k": pb30k.reshape(NT, 128, TS),
            "c_rc": rc, "c_cmb": cmb, "c_ident": np.eye(128, dtype=np.float32), "c_bones": bones}


def _pc(v, p=128):
    v = np.asarray(v, dtype=np.float32)
    return np.ascontiguousarray(np.swapaxes(v.reshape(*v.shape[:-1], v.shape[-1] // p, p), -1, -2))


DEV_SPECS = {
    "norm_mix_t": ((4, 128, 8), "mix"), "norm_ffn_t": ((4, 128, 8), "ffn"),
    "ffn_w_in": ((4, 1024, 5632), "ffn"), "ffn_w_out": ((4, 2816, 1024), "ffn"),
    "ffn_cw": ((4, 128, 3, 44), "ffn"), "ffn_cb": ((4, 128, 44), "ffn"),
    "rg_w_in": ((2, 1024, 2560), "rg"), "rg_w_out": ((2, 1280, 1024), "rg"),
    "rg_w_a": ((2, 10, 128, 128), "rg"), "rg_w_x": ((2, 10, 128, 128), "rg"),
    "rg_cw": ((2, 128, 4, 10), "rg"), "rg_vec": ((2, 128, 4, 10), "rg"),
    "pa_w_in": ((2, 1024, 2048), "pa"), "pa_w_out": ((2, 1024, 1024), "pa"), "pa_pool_w": ((2, 4, 128, 128), "pa"),
    "pa_vec": ((2, 128, 6), "pa"),
}


def const_specs(T):
    NT = T // TS
    return {"c_kaux": (8, 20, T), "c_qaux": (8, 4, T), "c_pbinf": (NT, 128, TS), "c_pb30k": (NT, 128, TS),
            "c_rc": (128, 4, TS), "c_cmb": (4, 128, TS), "c_ident": (128, 128), "c_bones": (128, 128)}


def prep_inputs(inputs):
    f = lambda k: np.ascontiguousarray(inputs[k], dtype=np.float32)
    d = {}
    d["norm_mix_t"] = _pc(f("norm_mix"))
    d["norm_ffn_t"] = _pc(f("norm_ffn"))
    for k in ("ffn_w_in", "ffn_w_out", "rg_w_in", "rg_w_out", "rg_w_a", "rg_w_x", "pa_w_in", "pa_w_out", "pa_pool_w"):
        d[k] = f(k)
    d["ffn_cw"] = np.ascontiguousarray(np.transpose(_pc(f("ffn_conv_w")), (0, 2, 1, 3)))
    d["ffn_cb"] = _pc(f("ffn_conv_b"))
    d["rg_cw"] = np.ascontiguousarray(np.transpose(_pc(f("rg_conv_w")), (0, 2, 1, 3)))
    d["rg_vec"] = np.ascontiguousarray(np.stack([_pc(f("rg_conv_b")), _pc(f("rg_b_a")), _pc(f("rg_b_x")), _pc(f("rg_lambda"))], axis=2))
    qg = np.concatenate([f("pa_q_gain"), f("pa_q_gain")], axis=1)
    kg = np.concatenate([f("pa_k_gain"), f("pa_k_gain")], axis=1)
    d["pa_vec"] = np.ascontiguousarray(np.concatenate([_pc(f("pa_pool_scale")), qg[:, :, None], kg[:, :, None]], axis=2))
    return d


def build(T=4096, plan=None):
    if plan is None:
        plan = []
        for l in range(4):
            plan.append(("pa" if l % 2 == 0 else "rg", l))
            plan.append(("ffn", l))
    nc = bass.Bass("TRN2", target_bir_lowering=False)
    xT = nc.dram_tensor("xT", [D, T], F32, kind="ExternalInput").ap()
    yT = nc.dram_tensor("yT", [D, T], F32, kind="ExternalOutput").ap()
    kinds = {k for k, _ in plan}
    if "pa" in kinds or "rg" in kinds:
        kinds.add("mix")
    used = [k for k, (shp, grp) in DEV_SPECS.items() if grp in kinds]
    P = {k: nc.dram_tensor(k, list(DEV_SPECS[k][0]), F32, kind="ExternalInput").ap() for k in used}
    scrd = {}
    if "ffn" in kinds:
        scrd["w2s"] = nc.dram_tensor("w2s", [NC_, 128, NJ, 128], BF16).ap()
    if "pa" in kinds:
        for k, shp in const_specs(T).items():
            P[k] = nc.dram_tensor(k, list(shp), F32, kind="ExternalInput").ap()
            used.append(k)
        scrd["qd"] = nc.dram_tensor("qd", [8, 84, T], BF16).ap()
        scrd["kd"] = nc.dram_tensor("kd", [8, 84, T], BF16).ap()
        scrd["vd"] = nc.dram_tensor("vd", [T, 512], BF16).ap()
        scrd["pad"] = nc.dram_tensor("pad", [512, T], BF16).ap()
    scr = [nc.dram_tensor(f"xs{i}", [D, T], F32).ap() for i in range(2)]
    with ExitStack() as es:
        kb = KB(nc, es)
        cst = {}
        cst["ones"] = alloc(es, nc, "c_ones", [128, 128], BF16)
        cst["eps"] = alloc(es, nc, "c_eps", [128, 1], F32)
        kb.op("pool", [], [cst["ones"].res], lambda e: e.memset(cst["ones"].t[:, :], 1.0))
        kb.op("pool", [], [cst["eps"].res], lambda e: e.memset(cst["eps"].t[:, :], EPS))
        cst["one"] = alloc(es, nc, "c_one", [128, 1], F32)
        kb.op("pool", [], [cst["one"].res], lambda e: e.memset(cst["one"].t[:, :], 1.0))
        if "pa" in kinds:
            cst["ident"] = alloc(es, nc, "c_identb", [128, 128], BF16)
            cst["bones"] = alloc(es, nc, "c_bonesb", [128, 128], BF16)
            cst["onesf"] = alloc(es, nc, "c_onesf", [65, 64], F32)
            kb.dma("pool", cst["ident"].t[:, :], P["c_ident"], [], [cst["ident"].res])
            kb.dma("pool", cst["bones"].t[:, :], P["c_bones"], [], [cst["bones"].res])
            kb.op("pool", [], [cst["onesf"].res], lambda e: e.memset(cst["onesf"].t[:, :], 1.0))
            scrd["aux_res"] = []
            for h in range(8):
                r1, r2 = Res(), Res()
                kb.dma("pool", scrd["kd"][h, 64:84, :], P["c_kaux"][h], [], [r1])
                kb.dma("pool", scrd["qd"][h, 80:84, :], P["c_qaux"][h], [], [r2])
                scrd["aux_res"] += [r1, r2]
        src = xT
        for i, (kind, l) in enumerate(plan):
            if i > 0:
                kb.barrier()
            dst = yT if i == len(plan) - 1 else scr[i % 2]
            if kind == "ffn":
                ffn_pass(kb, P, l, src, dst, T, cst, scrd)
            elif kind == "rg":
                rg_pass(kb, P, l, src, dst, T, cst)
            elif kind == "pa":
                pa_pass(kb, P, l, src, dst, T, cst, scrd)
            else:
                raise NotImplementedError(kind)
            src = dst
        for q, ring in kb.rings.items():
            for tok in ring["last"]:
                if tok is not None:
                    kb._wait("sp", tok)
        for e in ("pe", "act", "dve", "pool"):
            if kb.cnt[e]:
                kb._wait("sp", (e, kb.sem[e], kb.cnt[e]))
    kb.used = used
    return nc, kb


def kernel(**inputs):
    x = np.ascontiguousarray(inputs["x"], dtype=np.float32)
    B, S, _ = x.shape
    nc, kb = build(T=S)
    dev = prep_inputs(inputs)
    dev.update(make_consts(S))
    params = {k: dev[k] for k in kb.used}
    in_maps = []
    for b in range(B):
        m = dict(params)
        m["xT"] = np.ascontiguousarray(x[b].T)
        in_maps.append(m)
    res = run_bass_kernel_spmd(nc, in_maps, core_ids=list(range(B)))
    out = np.stack([np.ascontiguousarray(res.results[b]["yT"].T) for b in range(B)], axis=0)
    return out.astype(np.float32)
```

```python
from contextlib import ExitStack
import numpy as np
import concourse.bass as bass
import concourse.mybir as mybir
from concourse.bass_utils import run_bass_kernel_spmd

F32 = mybir.dt.float32
BF16 = mybir.dt.bfloat16
AF = mybir.ActivationFunctionType
ALU = mybir.AluOpType
AX = mybir.AxisListType

D = 1024
NC_ = 8
DFF = 2816
NJ = 22
RW = 1280
NR = 10
TS = 512
EPS = 1e-6
NEG = -30000.0


class Res:
    __slots__ = ("w", "r")

    def __init__(self):
        self.w = None
        self.r = {}


class KB:
    def __init__(self, nc, es):
        self.nc = nc
        self.engs = {"pe": nc.tensor, "act": nc.scalar, "dve": nc.vector, "pool": nc.gpsimd, "sp": nc.sync}
        self.sem = {}
        self.cnt = {}
        for e in ("pe", "act", "dve", "pool"):
            self.sem[e] = es.enter_context(nc.semaphore("s_" + e))
            self.cnt[e] = 0
        self.waited = {e: {} for e in self.engs}
        self.rings = {}
        for q, n in (("sp", 24), ("pool", 12), ("act", 4)):
            self.rings[q] = {
                "sems": [es.enter_context(nc.semaphore(f"d_{q}{i}")) for i in range(n)],
                "val": [0] * n, "last": [None] * n, "i": 0,
            }
        self.ninst = 0

    def _wait(self, eng, tok):
        key, sem, val = tok
        if self.waited[eng].get(key, 0) >= val:
            return
        self.engs[eng].wait_ge(sem, val)
        self.waited[eng][key] = val

    def deps(self, eng, reads, writes):
        for r in reads:
            if r.w is not None:
                self._wait(eng, r.w)
        for w in writes:
            if w.w is not None and w.w[0] != eng:
                self._wait(eng, w.w)
            for k, tok in w.r.items():
                if k != eng:
                    self._wait(eng, tok)

    def op(self, eng, reads, writes, fn):
        self.deps(eng, reads, writes)
        ins = fn(self.engs[eng])
        self.cnt[eng] += 1
        tok = (eng, self.sem[eng], self.cnt[eng])
        ins.then_inc(self.sem[eng], 1)
        for r in reads:
            r.r[eng] = tok
        for w in writes:
            w.w = tok
            w.r = {}
        self.ninst += 1
        return tok

    def dma(self, q, out, in_, reads, writes):
        self.deps(q, reads, writes)
        ring = self.rings[q]
        i = ring["i"]
        ring["i"] = (i + 1) % len(ring["sems"])
        if ring["last"][i] is not None:
            self._wait(q, ring["last"][i])
        ring["val"][i] += 16
        key = f"dma_{q}{i}"
        tok = (key, ring["sems"][i], ring["val"][i])
        self.engs[q].dma_start(out=out, in_=in_).then_inc(ring["sems"][i], 16)
        ring["last"][i] = tok
        for r in reads:
            r.r[key] = tok
        for w in writes:
            w.w = tok
            w.r = {}
        self.ninst += 1
        return tok

    def barrier(self):
        toks = [(e, self.sem[e], self.cnt[e]) for e in ("pe", "act", "dve", "pool") if self.cnt[e]]
        for ring in self.rings.values():
            toks += [t for t in ring["last"] if t is not None]
        for e in self.engs:
            for t in toks:
                self._wait(e, t)

    def wait_all(self, eng, toks):
        for t in toks:
            if t is not None:
                self._wait(eng, t)


class Buf:
    def __init__(self, t, nparts=0):
        self.t = t
        self.res = Res()
        self.p = [Res() for _ in range(nparts)]


_UID = [0]


def alloc(es, nc, name, shape, dt, nparts=0):
    _UID[0] += 1
    return Buf(es.enter_context(nc.sbuf_tensor(f"{name}_{_UID[0]}", list(shape), dt)), nparts)


def palloc(es, nc, name, shape, dt=F32):
    _UID[0] += 1
    return Buf(es.enter_context(nc.psum_tensor(f"{name}_{_UID[0]}", list(shape), dt)))


def rmsnorm_tile(kb, X, H, G, ones, SQ, RS, ps_n, n=TS, hoff=0):
    SQl = SQ if isinstance(SQ, list) else [SQ]
    L = len(SQl)

    def square(c):
        sq = SQl[c % L]
        kb.op("act", [X.res], [sq.res],
              lambda e: e.activation(out=sq.t[:, 0:n], in_=X.t[:, c, 0:n], func=AF.Square))

    for c in range(min(L, NC_)):
        square(c)
    for c in range(NC_):
        sq = SQl[c % L]
        kb.op("pe", [sq.res, ones.res], [ps_n.res],
              lambda e, c=c, sq=sq: e.matmul(ps_n.t[:, 0:n], ones.t[:, :], sq.t[:, 0:n], start=(c == 0), stop=(c == NC_ - 1)))
        if c + L < NC_:
            square(c + L)
    kb.op("act", [ps_n.res, G["eps"].res], [RS.res],
          lambda e: e.activation(out=RS.t[:, 0:n], in_=ps_n.t[:, 0:n], func=AF.Sqrt, bias=G["eps"].t[:, 0:1], scale=1.0 / D))
    kb.op("dve", [RS.res], [RS.res], lambda e: e.reciprocal(out=RS.t[:, 0:n], in_=RS.t[:, 0:n]))
    for c in range(NC_):
        kb.op("dve", [X.res, RS.res, G["g"].res], [H.res],
              lambda e, c=c: e.scalar_tensor_tensor(out=H.t[:, c, hoff:hoff + n], in0=X.t[:, c, 0:n], scalar=G["g"].t[:, c:c + 1],
                                                    in1=RS.t[:, 0:n], op0=ALU.mult, op1=ALU.mult))


def emit_skewed(chains):
    if not chains:
        return
    S = 1 + max(max(c.keys()) for c in chains)
    n = len(chains)
    for step in range(n + S - 1):
        for st in range(S - 1, -1, -1):
            ci = step - st
            if 0 <= ci < n and st in chains[ci]:
                chains[ci][st]()


def ffn_pass(kb, P, l, x_src, x_dst, T, cst, scr):
    nc = kb.nc
    NT = T // TS
    w2s = scr["w2s"]
    with ExitStack() as es:
        NBLK = 8
        BW = DFF // NBLK
        W1 = alloc(es, nc, "f_w1", [128, NC_, 2 * DFF], BF16, NC_ * 2 * NBLK)
        W2S = [alloc(es, nc, f"f_w2s{i}", [128, NJ, 128], BF16) for i in range(2)]
        CW = alloc(es, nc, "f_cw", [128, 3, 2 * NJ], F32)
        CB = alloc(es, nc, "f_cb", [128, 2 * NJ], F32)
        Gg = alloc(es, nc, "f_g", [128, NC_], F32)
        TAIL = alloc(es, nc, "f_tail", [128, 2 * NJ, 2], F32)
        X = [alloc(es, nc, f"f_x{i}", [128, NC_, TS], F32) for i in range(2)]
        H = [alloc(es, nc, f"f_h{i}", [128, NC_, TS], BF16) for i in range(2)]
        ACTB = [alloc(es, nc, f"f_act{j}", [128, TS], BF16) for j in range(NJ)]
        ZS = [[alloc(es, nc, f"f_zs{i}_{u}", [128, TS + 2], F32, 1) for u in range(2)] for i in range(2)]
        CZ = [[alloc(es, nc, f"f_cz{i}_{u}", [128, TS], F32) for u in range(2)] for i in range(4)]
        GG = [alloc(es, nc, f"f_gg{i}", [128, TS], F32) for i in range(2)]
        SQ = [alloc(es, nc, f"f_sq{i}", [128, TS], BF16) for i in range(4)]
        RS = alloc(es, nc, "f_rs", [128, TS], F32)
        ps_z = [palloc(es, nc, f"f_psz{i}", [128, TS]) for i in range(4)]
        ps_o = [palloc(es, nc, f"f_pso{i}", [128, TS]) for i in range(2)]
        ps_n = palloc(es, nc, "f_psn", [128, TS])
        G = {"g": Gg, "eps": cst["eps"]}

        w_in = P["ffn_w_in"]
        for blk in range(NBLK):
            for ug in range(2):
                c0 = ug * DFF + blk * BW
                for c in range(NC_):
                    kb.dma("pool", W1.t[:, c, c0:c0 + BW], w_in[l, c * 128:(c + 1) * 128, c0:c0 + BW], [],
                           [W1.p[(ug * NBLK + blk) * NC_ + c]])
        r_w2 = [Res() for _ in range(NC_)]
        for m in range(NC_):
            kb.dma("pool", w2s[m], P["ffn_w_out"][l, :, m * 128:(m + 1) * 128].rearrange("(j p) n -> p j n", p=128), [], [r_w2[m]])
        kb.dma("sp", CW.t[:, :, :], P["ffn_cw"][l], [], [CW.res])
        kb.dma("sp", CB.t[:, :], P["ffn_cb"][l], [], [CB.res])
        kb.dma("sp", Gg.t[:, :], P["norm_ffn_t"][l], [], [Gg.res])
        kb.op("pool", [], [TAIL.res], lambda e: e.memset(TAIL.t[:, :, :], 0.0))

        chains = []
        kc = [0]

        def prologue(n):
            def f():
                t0 = n * TS
                Xn, Hn = X[n % 2], H[n % 2]
                kb.dma("sp", Xn.t[:, :, :], x_src[:, t0:t0 + TS].rearrange("(c p) t -> p c t", p=128), [], [Xn.res])
                rmsnorm_tile(kb, Xn, Hn, G, cst["ones"], SQ, RS, ps_n)
            return {0: f}

        def chain(n, j):
            k = kc[0]
            kc[0] += 1
            Hn = H[n % 2]
            pss = (ps_z[(k % 2) * 2], ps_z[(k % 2) * 2 + 1])
            zs2, cz2, gg = ZS[k % 2], CZ[k % 4], GG[k % 2]

            def s0():
                for ug in range(2):
                    jj = ug * NJ + j
                    ps = pss[ug]
                    blks = sorted({(j * 128) // BW, (j * 128 + 127) // BW})
                    for c in range(NC_):
                        kb.op("pe", [W1.p[(ug * NBLK + b) * NC_ + c] for b in blks] + [Hn.res], [ps.res],
                              lambda e, c=c, jj=jj, ps=ps: e.matmul(ps.t[:, :], W1.t[:, c, jj * 128:(jj + 1) * 128], Hn.t[:, c, :],
                                                                    start=(c == 0), stop=(c == NC_ - 1)))

            def s1():
                for ug in range(2):
                    jj = ug * NJ + j
                    zs = zs2[ug]
                    kb.op("pool", [TAIL.res], [zs.p[0]], lambda e, zs=zs, jj=jj: e.tensor_copy(out=zs.t[:, 0:2], in_=TAIL.t[:, jj, :]))
                for ug in range(2):
                    jj = ug * NJ + j
                    ps, zs, cz = pss[ug], zs2[ug], cz2[ug]
                    kb.op("act", [ps.res], [zs.res], lambda e, zs=zs, ps=ps: e.copy(out=zs.t[:, 2:TS + 2], in_=ps.t[:, :]))
                    kb.op("act", [zs.res, CW.res, CB.res], [cz.res],
                          lambda e, zs=zs, cz=cz, jj=jj: e.activation(out=cz.t[:, :], in_=zs.t[:, 2:TS + 2], func=AF.Identity,
                                                                      bias=CB.t[:, jj:jj + 1], scale=CW.t[:, 2, jj:jj + 1]))
                for ug in range(2):
                    jj = ug * NJ + j
                    zs = zs2[ug]
                    kb.op("pool", [zs.res], [TAIL.res], lambda e, zs=zs, jj=jj: e.tensor_copy(out=TAIL.t[:, jj, :], in_=zs.t[:, TS:TS + 2]))

            def s2():
                for ug in range(2):
                    jj = ug * NJ + j
                    zs, cz = zs2[ug], cz2[ug]
                    for kk in (1, 0):
                        kb.op("dve", [zs.res, zs.p[0], cz.res, CW.res], [cz.res],
                              lambda e, zs=zs, cz=cz, jj=jj, kk=kk: e.scalar_tensor_tensor(
                                  out=cz.t[:, :], in0=zs.t[:, kk:kk + TS], scalar=CW.t[:, kk, jj:jj + 1], in1=cz.t[:, :],
                                  op0=ALU.mult, op1=ALU.add))

            def s3():
                kb.op("act", [cz2[1].res], [gg.res], lambda e: e.activation(out=gg.t[:, :], in_=cz2[1].t[:, :], func=AF.Gelu_apprx_tanh))

            def s4():
                kb.op("dve", [gg.res, cz2[0].res], [ACTB[j].res],
                      lambda e: e.tensor_tensor(out=ACTB[j].t[:, :], in0=gg.t[:, :], in1=cz2[0].t[:, :], op=ALU.mult))

            return {0: s0, 1: s1, 2: s2, 3: s3, 4: s4}

        mcount = [0]

        def epilogue(n):
            def f():
                t0 = n * TS
                Xn = X[n % 2]
                for m in range(NC_):
                    wb = W2S[mcount[0] % 2]
                    mcount[0] += 1
                    kb.dma("sp", wb.t[:, :, :], w2s[m], [r_w2[m]], [wb.res])
                    ps = ps_o[m % 2]
                    for j in range(NJ):
                        kb.op("pe", [wb.res, ACTB[j].res], [ps.res],
                              lambda e, j=j, ps=ps, wb=wb: e.matmul(ps.t[:, :], wb.t[:, j, :], ACTB[j].t[:, :], start=(j == 0), stop=(j == NJ - 1)))
                    kb.op("dve", [ps.res, Xn.res], [Xn.res],
                          lambda e, m=m, ps=ps: e.tensor_tensor(out=Xn.t[:, m, :], in0=ps.t[:, :], in1=Xn.t[:, m, :], op=ALU.add))
                kb.dma("sp", x_dst[:, t0:t0 + TS].rearrange("(c p) t -> p c t", p=128), Xn.t[:, :, :], [Xn.res], [])
            return {4: f}

        for n in range(NT):
            chains.append(prologue(n))
            for j in range(NJ):
                chains.append(chain(n, j))
            chains.append(epilogue(n))
        emit_skewed(chains)


def rg_pass(kb, P, l, x_src, x_dst, T, cst):
    nc = kb.nc
    NT = T // TS
    jl = l // 2
    with ExitStack() as es:
        W1 = alloc(es, nc, "r_w1", [128, NC_, 2 * RW], BF16, NC_)
        W2 = alloc(es, nc, "r_w2", [128, NR, D], BF16, NR)
        WA = alloc(es, nc, "r_wa", [128, NR, 128], BF16)
        WX = alloc(es, nc, "r_wx", [128, NR, 128], BF16)
        CW = alloc(es, nc, "r_cw", [128, 4, NR], F32)
        VEC = alloc(es, nc, "r_vec", [128, 4, NR], F32)
        C8 = alloc(es, nc, "r_c8", [128, NR], F32)
        C16 = alloc(es, nc, "r_c16", [128, NR], F32)
        Gg = alloc(es, nc, "r_g", [128, NC_], F32)
        TAIL = alloc(es, nc, "r_tail", [128, NR, 3], F32)
        STATE = alloc(es, nc, "r_state", [128, NR], F32)
        X = [alloc(es, nc, f"r_x{i}", [128, NC_, TS], F32) for i in range(2)]
        H = [alloc(es, nc, f"r_h{i}", [128, NC_, TS], BF16) for i in range(2)]
        Y = [[alloc(es, nc, f"r_y{i}_{j}", [128, TS], BF16) for j in range(NR)] for i in range(2)]

        def mk(nm, cnt, dt=F32, w=TS):
            return [alloc(es, nc, f"r_{nm}{i}", [128, w], dt, 1) for i in range(cnt)]
        GGs, XCs, XSs, XCBs = mk("gg", 5), mk("xc", 5), mk("xs", 2, w=TS + 3), mk("xcb", 2, BF16)
        Rs, Is, As, Ss, Bs, HSs = mk("r", 2), mk("i", 2), mk("a", 2), mk("s", 2), mk("b", 2), mk("hs", 2)
        SQ = [alloc(es, nc, f"r_sq{i}", [128, TS], BF16) for i in range(8)]
        RS = alloc(es, nc, "r_rs", [128, TS], F32)
        ps_gx = [palloc(es, nc, f"r_psgx{i}", [128, TS]) for i in range(4)]
        ps_ri = [palloc(es, nc, f"r_psri{i}", [128, TS]) for i in range(2)]
        ps_o = palloc(es, nc, "r_pso", [128, TS])
        ps_n = palloc(es, nc, "r_psn", [128, TS])
        G = {"g": Gg, "eps": cst["eps"]}

        for c in range(NC_):
            kb.dma("pool", W1.t[:, c, :], P["rg_w_in"][jl, c * 128:(c + 1) * 128, :], [], [W1.p[c]])
        for j in range(NR):
            kb.dma("pool", W2.t[:, j, :], P["rg_w_out"][jl, j * 128:(j + 1) * 128, :], [], [W2.p[j]])
        kb.dma("pool", WA.t[:, :, :], P["rg_w_a"][jl].rearrange("n c d -> c n d"), [], [WA.res])
        kb.dma("pool", WX.t[:, :, :], P["rg_w_x"][jl].rearrange("n c d -> c n d"), [], [WX.res])
        kb.dma("sp", CW.t[:, :, :], P["rg_cw"][jl], [], [CW.res])
        kb.dma("sp", VEC.t[:, :, :], P["rg_vec"][jl], [], [VEC.res])
        kb.dma("sp", Gg.t[:, :], P["norm_mix_t"][l], [], [Gg.res])
        kb.op("pool", [], [TAIL.res], lambda e: e.memset(TAIL.t[:, :, :], 0.0))
        kb.op("pool", [], [STATE.res], lambda e: e.memset(STATE.t[:, :], 0.0))
        kb.op("act", [VEC.res], [C8.res], lambda e: e.activation(out=C8.t[:, :], in_=VEC.t[:, 3, :], func=AF.Exp, scale=-1.0))
        kb.op("act", [C8.res, cst["one"].res], [C8.res],
              lambda e: e.activation(out=C8.t[:, :], in_=C8.t[:, :], func=AF.Ln, bias=cst["one"].t[:, 0:1], scale=1.0))
        kb.op("dve", [C8.res], [C16.res], lambda e: e.tensor_scalar(out=C16.t[:, :], in0=C8.t[:, :], scalar1=-16.0, scalar2=None, op0=ALU.mult))
        kb.op("dve", [C8.res], [C8.res], lambda e: e.tensor_scalar(out=C8.t[:, :], in0=C8.t[:, :], scalar1=-8.0, scalar2=None, op0=ALU.mult))

        kc = [0]

        def prologue(n):
            def f():
                t0 = n * TS
                Xn, Hn = X[n % 2], H[n % 2]
                kb.dma("sp", Xn.t[:, :, :], x_src[:, t0:t0 + TS].rearrange("(c p) t -> p c t", p=128), [], [Xn.res])
                rmsnorm_tile(kb, Xn, Hn, G, cst["ones"], SQ, RS, ps_n)
            return {0: f}

        def chain(n, j):
            k = kc[0]
            kc[0] += 1
            Hn = H[n % 2]
            pg, px = ps_gx[(k % 2) * 2], ps_gx[(k % 2) * 2 + 1]
            gg, xc, xs, xcb = GGs[k % 5], XCs[k % 5], XSs[k % 2], XCBs[k % 2]
            r, ii, a, sq_, b, hs = Rs[k % 2], Is[k % 2], As[k % 2], Ss[k % 2], Bs[k % 2], HSs[k % 2]
            Yj = Y[n % 2][j]

            def s0():
                for (ps, off) in ((pg, j * 128), (px, RW + j * 128)):
                    for c in range(NC_):
                        kb.op("pe", [W1.p[c], Hn.res], [ps.res],
                              lambda e, c=c, ps=ps, off=off: e.matmul(ps.t[:, :], W1.t[:, c, off:off + 128], Hn.t[:, c, :],
                                                                      start=(c == 0), stop=(c == NC_ - 1)))

            def s1():
                kb.op("pool", [TAIL.res], [xs.p[0]], lambda e: e.tensor_copy(out=xs.t[:, 0:3], in_=TAIL.t[:, j, :]))
                kb.op("dve", [px.res], [xs.res], lambda e: e.tensor_copy(out=xs.t[:, 3:TS + 3], in_=px.t[:, :]))
                kb.op("pool", [xs.res], [TAIL.res], lambda e: e.tensor_copy(out=TAIL.t[:, j, :], in_=xs.t[:, TS:TS + 3]))
                kb.op("act", [xs.res, CW.res, VEC.res], [xc.res],
                      lambda e: e.activation(out=xc.t[:, :], in_=xs.t[:, 3:TS + 3], func=AF.Identity,
                                             bias=VEC.t[:, 0, j:j + 1], scale=CW.t[:, 3, j:j + 1]))
                kb.op("act", [pg.res], [gg.res], lambda e: e.activation(out=gg.t[:, :], in_=pg.t[:, :], func=AF.Gelu_apprx_tanh))

            def s2():
                for kk in (2, 1, 0):
                    kb.op("dve", [xs.res, xs.p[0], xc.res, CW.res], [xc.res],
                          lambda e, kk=kk: e.scalar_tensor_tensor(
                              out=xc.t[:, :], in0=xs.t[:, kk:kk + TS], scalar=CW.t[:, kk, j:j + 1], in1=xc.t[:, :], op0=ALU.mult, op1=ALU.add))
                kb.op("pool", [xc.res], [xcb.res], lambda e: e.tensor_copy(out=xcb.t[:, :], in_=xc.t[:, :]))

            def s3():
                kb.op("pe", [WA.res, xcb.res], [ps_ri[0].res],
                      lambda e: e.matmul(ps_ri[0].t[:, :], WA.t[:, j, :], xcb.t[:, :], start=True, stop=True))
                kb.op("pe", [WX.res, xcb.res], [ps_ri[1].res],
                      lambda e: e.matmul(ps_ri[1].t[:, :], WX.t[:, j, :], xcb.t[:, :], start=True, stop=True))

            def s4():
                kb.op("act", [ps_ri[0].res, VEC.res], [r.res],
                      lambda e: e.activation(out=r.t[:, :], in_=ps_ri[0].t[:, :], func=AF.Sigmoid, bias=VEC.t[:, 1, j:j + 1], scale=1.0))
                kb.op("act", [ps_ri[1].res, VEC.res], [ii.res],
                      lambda e: e.activation(out=ii.t[:, :], in_=ps_ri[1].t[:, :], func=AF.Sigmoid, bias=VEC.t[:, 2, j:j + 1], scale=1.0))
                kb.op("act", [r.res, C8.res], [a.res],
                      lambda e: e.activation(out=a.t[:, :], in_=r.t[:, :], func=AF.Exp, scale=C8.t[:, j:j + 1]))
                kb.op("act", [r.res, C16.res], [sq_.res],
                      lambda e: e.activation(out=sq_.t[:, :], in_=r.t[:, :], func=AF.Exp, scale=C16.t[:, j:j + 1]))
                kb.op("act", [sq_.res, cst["one"].res], [sq_.res],
                      lambda e: e.activation(out=sq_.t[:, :], in_=sq_.t[:, :], func=AF.Sqrt, bias=cst["one"].t[:, 0:1], scale=-1.0))

            def s5():
                kb.op("dve", [ii.res, xc.res], [b.res], lambda e: e.tensor_tensor(out=b.t[:, :], in0=ii.t[:, :], in1=xc.t[:, :], op=ALU.mult))
                kb.op("dve", [b.res, sq_.res], [b.res], lambda e: e.tensor_tensor(out=b.t[:, :], in0=b.t[:, :], in1=sq_.t[:, :], op=ALU.mult))
                kb.op("dve", [a.res, b.res, STATE.res], [hs.res],
                      lambda e: e.tensor_tensor_scan(out=hs.t[:, :], data0=a.t[:, :], data1=b.t[:, :],
                                                     initial=STATE.t[:, j:j + 1], op0=ALU.mult, op1=ALU.add))
                kb.op("pool", [hs.res], [STATE.res], lambda e: e.tensor_copy(out=STATE.t[:, j:j + 1], in_=hs.t[:, TS - 1:TS]))
                kb.op("dve", [hs.res, gg.res], [Yj.res], lambda e: e.tensor_tensor(out=Yj.t[:, :], in0=gg.t[:, :], in1=hs.t[:, :], op=ALU.mult))

            return {0: s0, 1: s1, 2: s2, 3: s3, 4: s4, 5: s5}

        def epilogue(n):
            def f():
                t0 = n * TS
                Xn = X[n % 2]
                for m in range(NC_):
                    for j in range(NR):
                        Yj = Y[n % 2][j]
                        kb.op("pe", [W2.p[j], Yj.res], [ps_o.res],
                              lambda e, m=m, j=j, Yj=Yj: e.matmul(ps_o.t[:, :], W2.t[:, j, m * 128:(m + 1) * 128], Yj.t[:, :],
                                                                  start=(j == 0), stop=(j == NR - 1)))
                    kb.op("dve", [ps_o.res, Xn.res], [Xn.res],
                          lambda e, m=m: e.tensor_tensor(out=Xn.t[:, m, :], in0=ps_o.t[:, :], in1=Xn.t[:, m, :], op=ALU.add))
                kb.dma("sp", x_dst[:, t0:t0 + TS].rearrange("(c p) t -> p c t", p=128), Xn.t[:, :, :], [Xn.res], [])
            return {5: f}

        chains = []
        for n in range(NT):
            chains.append(prologue(n))
            for j in range(NR):
                chains.append(chain(n, j))
            chains.append(epilogue(n))
        emit_skewed(chains)


def pa_pass(kb, P, l, x_src, x_dst, T, cst, scr):
    nc = kb.nc
    NT = T // TS
    jl = l // 2
    qd, kd, vd, pad = scr["qd"], scr["kd"], scr["vd"], scr["pad"]
    r_q = [[] for _ in range(NT)]
    r_k = [[] for _ in range(NT)]
    r_v = [[] for _ in range(NT)]
    r_p = [[] for _ in range(NT)]
    aux = scr["aux_res"]

    def nr(lst):
        r = Res()
        lst.append(r)
        return [r]

    def cat(lsts):
        out = []
        for x in lsts:
            out += x
        return out
    with ExitStack() as es:
        WIN = alloc(es, nc, "p_win", [128, NC_, 2048], BF16, NC_)
        PW = alloc(es, nc, "p_pw", [128, 4, 128], BF16)
        VEC = alloc(es, nc, "p_vec", [128, 6], F32)
        QG8 = alloc(es, nc, "p_qg8", [128, 1], F32)
        Gg = alloc(es, nc, "p_g", [128, NC_], F32)
        RC = alloc(es, nc, "p_rc", [128, 4, TS], F32)
        PBI = alloc(es, nc, "p_pbi", [128, TS], F32)
        PB3 = alloc(es, nc, "p_pb3", [128, TS], F32)
        PTAIL = alloc(es, nc, "p_ptail", [128, 4, 16], F32)
        KM = alloc(es, nc, "p_km", [128, 4, 16], F32)
        X = alloc(es, nc, "p_x", [128, NC_, TS], F32)
        H = alloc(es, nc, "p_h", [128, NC_, TS], BF16)
        US = [alloc(es, nc, f"p_us{i}", [128, TS + 16], F32) for i in range(2)]
        SA = alloc(es, nc, "p_sa", [128, TS + 16], F32)
        SB = alloc(es, nc, "p_sb", [128, TS + 16], F32)
        PBF = [alloc(es, nc, f"p_pbf{i}", [128, TS], BF16) for i in range(2)]
        PAo = [alloc(es, nc, f"p_pao{i}", [128, TS], BF16) for i in range(2)]
        SQh = [alloc(es, nc, f"p_sqh{i}", [128, TS], BF16) for i in range(2)]
        RSh = [alloc(es, nc, f"p_rsh{i}", [128, TS], F32) for i in range(2)]
        NF = [alloc(es, nc, f"p_nf{i}", [128, TS], F32) for i in range(2)]
        NB = [alloc(es, nc, f"p_nb{i}", [128, TS], BF16) for i in range(2)]
        KS = alloc(es, nc, "p_ks", [128, 2], F32)
        GP = alloc(es, nc, "p_gp", [128, TS], F32)
        M8 = alloc(es, nc, "p_m8", [128, 32, 8], F32)
        MBF = alloc(es, nc, "p_mbf", [128, TS], F32)
        MBB = alloc(es, nc, "p_mbb", [128, TS], BF16)
        MT = [alloc(es, nc, f"p_mt{i}", [16, TS], BF16) for i in range(2)]
        VT = [alloc(es, nc, f"p_vt{i}", [128, TS], BF16) for i in range(2)]
        SQ = [alloc(es, nc, f"p_sq{i}", [128, TS], BF16) for i in range(8)]
        RS = alloc(es, nc, "p_rs", [128, TS], F32)
        ps_n = palloc(es, nc, "p_psn", [128, TS])
        ps_a = [palloc(es, nc, f"p_psa{i}", [128, TS]) for i in range(2)]
        ps_ms = palloc(es, nc, "p_psms", [128, TS])
        ps_pw = palloc(es, nc, "p_pspw", [128, TS])
        ps_g = palloc(es, nc, "p_psg", [128, TS])
        ps_t = palloc(es, nc, "p_pst", [16, TS], BF16)
        G = {"g": Gg, "eps": cst["eps"]}

        for c in range(NC_):
            kb.dma("pool", WIN.t[:, c, :], P["pa_w_in"][jl, c * 128:(c + 1) * 128, :], [], [WIN.p[c]])
        kb.dma("pool", PW.t[:, :, :], P["pa_pool_w"][jl].rearrange("g c d -> c g d"), [], [PW.res])
        kb.dma("sp", VEC.t[:, :], P["pa_vec"][jl], [], [VEC.res])
        kb.dma("sp", Gg.t[:, :], P["norm_mix_t"][l], [], [Gg.res])
        kb.dma("sp", RC.t[:, :, :], P["c_rc"], [], [RC.res])
        kb.op("dve", [VEC.res], [QG8.res], lambda e: e.tensor_scalar(out=QG8.t[:, :], in0=VEC.t[:, 4:5], scalar1=0.125, scalar2=None, op0=ALU.mult))
        kb.op("pool", [], [PTAIL.res], lambda e: e.memset(PTAIL.t[:, :, :], 0.0))
        kb.op("pool", [], [KM.res], lambda e: e.memset(KM.t[:, :, :], 0.0))

        def head_norm(ps, gcol, i):
            kb.op("act", [ps.res], [SQh[i].res], lambda e: e.activation(out=SQh[i].t[:, :], in_=ps.t[:, :], func=AF.Square))
            kb.op("pe", [SQh[i].res, cst["bones"].res], [ps_ms.res],
                  lambda e: e.matmul(ps_ms.t[:, :], cst["bones"].t[:, :], SQh[i].t[:, :], start=True, stop=True))
            kb.op("act", [ps_ms.res, cst["eps"].res], [RSh[i].res],
                  lambda e: e.activation(out=RSh[i].t[:, :], in_=ps_ms.t[:, :], func=AF.Sqrt, bias=cst["eps"].t[:, 0:1], scale=1.0 / 64))
            kb.op("dve", [RSh[i].res], [RSh[i].res], lambda e: e.reciprocal(out=RSh[i].t[:, :], in_=RSh[i].t[:, :]))
            kb.op("dve", [ps.res, RSh[i].res, VEC.res], [NF[i].res],
                  lambda e: e.scalar_tensor_tensor(out=NF[i].t[:, :], in0=ps.t[:, :], scalar=VEC.t[:, gcol:gcol + 1], in1=RSh[i].t[:, :],
                                                   op0=ALU.mult, op1=ALU.mult))

        def proj(ps, off):
            for c in range(NC_):
                kb.op("pe", [WIN.p[c], H.res], [ps.res],
                      lambda e, c=c: e.matmul(ps.t[:, :], WIN.t[:, c, off:off + 128], H.t[:, c, :], start=(c == 0), stop=(c == NC_ - 1)))

        ia = 0
        for n in range(NT):
            t0 = n * TS
            kb.dma("sp", X.t[:, :, :], x_src[:, t0:t0 + TS].rearrange("(c p) t -> p c t", p=128), [], [X.res])
            kb.dma("sp", PBI.t[:, :], P["c_pbinf"][n], [], [PBI.res])
            kb.dma("sp", PB3.t[:, :], P["c_pb30k"][n], [], [PB3.res])
            rmsnorm_tile(kb, X, H, G, cst["ones"], SQ, RS, ps_n)
            for g in range(4):
                ps = ps_a[ia % 2]; ia += 1
                proj(ps, g * 128)
                us = US[g % 2]
                kb.op("pool", [PTAIL.res], [us.res], lambda e, us=us, g=g: e.tensor_copy(out=us.t[:, 0:16], in_=PTAIL.t[:, g, :]))
                kb.op("act", [ps.res], [us.res], lambda e, us=us, ps=ps: e.copy(out=us.t[:, 16:TS + 16], in_=ps.t[:, :]))
                kb.op("pool", [us.res], [PTAIL.res], lambda e, us=us, g=g: e.tensor_copy(out=PTAIL.t[:, g, :], in_=us.t[:, TS:TS + 16]))
                src_b, step, L = us, 1, TS + 16
                bufs = [SA, SB]
                bi = 0
                lo = 0
                for _ in range(g + 1):
                    dst_b = bufs[bi]; bi ^= 1
                    nlo = lo + step
                    kb.op("dve", [src_b.res], [dst_b.res],
                          lambda e, src_b=src_b, dst_b=dst_b, nlo=nlo, step=step: e.tensor_tensor(
                              out=dst_b.t[:, nlo:L], in0=src_b.t[:, nlo:L], in1=src_b.t[:, nlo - step:L - step], op=ALU.add))
                    src_b, lo, step = dst_b, nlo, step * 2
                w = 2 ** (g + 1)
                pb = PBF[g % 2]
                if n == 0:
                    kb.op("dve", [src_b.res, RC.res], [src_b.res],
                          lambda e, src_b=src_b, g=g: e.tensor_tensor(out=src_b.t[:, 16:L], in0=src_b.t[:, 16:L], in1=RC.t[:, g, :], op=ALU.mult))
                    kb.op("dve", [src_b.res, us.res], [pb.res],
                          lambda e, src_b=src_b, us=us, pb=pb: e.tensor_tensor(out=pb.t[:, :], in0=src_b.t[:, 16:L], in1=us.t[:, 16:L], op=ALU.subtract))
                else:
                    kb.op("dve", [src_b.res, us.res], [pb.res],
                          lambda e, src_b=src_b, us=us, pb=pb, w=w: e.scalar_tensor_tensor(
                              out=pb.t[:, :], in0=src_b.t[:, 16:L], scalar=1.0 / w, in1=us.t[:, 16:L], op0=ALU.mult, op1=ALU.subtract))
                kb.op("pe", [PW.res, pb.res], [ps_pw.res],
                      lambda e, g=g, pb=pb: e.matmul(ps_pw.t[:, :], PW.t[:, g, :], pb.t[:, :], start=True, stop=True))
                po = PAo[g % 2]
                kb.op("act", [ps_pw.res, VEC.res], [po.res],
                      lambda e, g=g, po=po: e.activation(out=po.t[:, :], in_=ps_pw.t[:, :], func=AF.Identity, bias=0.0, scale=VEC.t[:, g:g + 1]))
                kb.dma("sp", pad[g * 128:(g + 1) * 128, t0:t0 + TS], po.t[:, :], [po.res], nr(r_p[n]))
            for pr in range(4):
                ps = ps_a[ia % 2]; ia += 1
                proj(ps, 1024 + pr * 128)
                i = pr % 2
                head_norm(ps, 5, i)
                kb.op("pool", [NF[i].res], [NB[i].res], lambda e, i=i: e.tensor_copy(out=NB[i].t[:, :], in_=NF[i].t[:, :]))
                for hh in range(2):
                    kb.dma("sp", kd[2 * pr + hh, 0:64, t0:t0 + TS], NB[i].t[hh * 64:(hh + 1) * 64, :], [NB[i].res], nr(r_k[n]))
                kb.op("dve", [NF[i].res], [KS.res],
                      lambda e, i=i: e.tensor_reduce(out=KS.t[:, :], in_=NF[i].t[:, :].rearrange("p (b t) -> p b t", b=2), axis=AX.X, op=ALU.add))
                kb.op("dve", [KS.res], [KM.res],
                      lambda e, pr=pr, n=n: e.tensor_scalar(out=KM.t[:, pr, 2 * n:2 * n + 2], in0=KS.t[:, :], scalar1=1.0 / 256, scalar2=None, op0=ALU.mult))
            for pr in range(4):
                ps = ps_a[ia % 2]; ia += 1
                proj(ps, 512 + pr * 128)
                i = pr % 2
                head_norm(ps, 4, i)
                kb.op("act", [NF[i].res], [NB[i].res], lambda e, i=i: e.mul(out=NB[i].t[:, :], in_=NF[i].t[:, :], mul=0.125))
                for hh in range(2):
                    kb.dma("sp", qd[2 * pr + hh, 0:64, t0:t0 + TS], NB[i].t[hh * 64:(hh + 1) * 64, :], [NB[i].res], nr(r_q[n]))
                for sub in range(4):
                    for hh in range(2):
                        hd = 2 * pr + hh
                        o = (sub * 8 + hd) * 16
                        kb.op("pe", [NF[i].res, KM.res], [ps_g.res],
                              lambda e, i=i, sub=sub, hh=hh, pr=pr, o=o: e.matmul(
                                  ps_g.t[:, o:o + 16], NF[i].t[hh * 64:(hh + 1) * 64, sub * 128:(sub + 1) * 128],
                                  KM.t[hh * 64:(hh + 1) * 64, pr, :], start=True, stop=True))
            kb.op("dve", [ps_g.res, PBI.res], [GP.res], lambda e: e.tensor_tensor(out=GP.t[:, :], in0=ps_g.t[:, :], in1=PBI.t[:, :], op=ALU.add))
            for sh in range(32):
                kb.op("dve", [GP.res], [M8.res], lambda e, sh=sh: e.max(out=M8.t[:, sh, :], in_=GP.t[:, sh * 16:(sh + 1) * 16]))
            for sh in range(32):
                kb.op("dve", [GP.res, M8.res], [MBF.res],
                      lambda e, sh=sh: e.tensor_scalar(out=MBF.t[:, sh * 16:(sh + 1) * 16], in0=GP.t[:, sh * 16:(sh + 1) * 16],
                                                       scalar1=M8.t[:, sh, 3:4], scalar2=-NEG, op0=ALU.is_ge, op1=ALU.mult))
            kb.op("dve", [MBF.res, PB3.res], [MBB.res], lambda e: e.tensor_tensor(out=MBB.t[:, :], in0=MBF.t[:, :], in1=PB3.t[:, :], op=ALU.add))
            for hd in range(8):
                for sub in range(4):
                    o = (sub * 8 + hd) * 16
                    kb.op("pe", [MBB.res, cst["ident"].res], [ps_t.res],
                          lambda e, sub=sub, o=o: e.transpose(ps_t.t[:, sub * 128:(sub + 1) * 128], MBB.t[:, o:o + 16], cst["ident"].t[:, :]))
                mt = MT[hd % 2]
                kb.op("act", [ps_t.res], [mt.res], lambda e, mt=mt: e.copy(out=mt.t[:, :], in_=ps_t.t[:, :]))
                kb.dma("sp", qd[hd, 64:80, t0:t0 + TS], mt.t[:, :], [mt.res], nr(r_q[n]))
            for sub in range(4):
                ps = ps_a[ia % 2]; ia += 1
                for c in range(NC_):
                    kb.op("pe", [WIN.p[c], H.res], [ps.res],
                          lambda e, c=c, sub=sub, ps=ps: e.matmul(ps.t[:, :], H.t[:, c, sub * 128:(sub + 1) * 128], WIN.t[:, c, 1536:2048],
                                                                  start=(c == 0), stop=(c == NC_ - 1)))
                vt = VT[sub % 2]
                kb.op("act", [ps.res], [vt.res], lambda e, vt=vt, ps=ps: e.copy(out=vt.t[:, :], in_=ps.t[:, :]))
                kb.dma("sp", vd[t0 + sub * 128:t0 + (sub + 1) * 128, :], vt.t[:, :], [vt.res], nr(r_v[n]))
    kb.barrier()
    with ExitStack() as es:
        WOP = alloc(es, nc, "a_wop", [128, 4, D], BF16, 4)
        WOA = alloc(es, nc, "a_woa", [64, 8, D], BF16, 8)
        CMB = alloc(es, nc, "a_cmb", [128, 4, TS], BF16)
        X = [alloc(es, nc, f"a_x{i}", [128, NC_, TS], F32) for i in range(2)]
        PAg = [alloc(es, nc, f"a_pag{i}", [128, 4, TS], BF16) for i in range(2)]
        QP = [alloc(es, nc, f"a_qp{i}", [84, TS], BF16) for i in range(2)]
        KP = [alloc(es, nc, f"a_kp{i}", [84, T], BF16) for i in range(2)]
        VS = [alloc(es, nc, f"a_vs{i}", [128, T // 128, 65], BF16) for i in range(2)]
        PT = [alloc(es, nc, f"a_pt{i}", [128, TS], BF16) for i in range(4)]
        RD = alloc(es, nc, "a_rd", [65, TS], F32)
        BC = alloc(es, nc, "a_bc", [64, TS], F32)
        ATT = [[alloc(es, nc, f"a_att{i}_{h}", [64, TS], BF16) for h in range(8)] for i in range(2)]
        ps_s = [palloc(es, nc, f"a_pss{i}", [128, TS]) for i in range(3)]
        ps_acc = [palloc(es, nc, f"a_psacc{i}", [65, TS]) for i in range(2)]
        ps_b = palloc(es, nc, "a_psb", [64, TS])
        ps_o = [palloc(es, nc, f"a_pso{i}", [128, TS]) for i in range(2)]

        for g in range(4):
            kb.dma("pool", WOP.t[:, g, :], P["pa_w_out"][jl, g * 128:(g + 1) * 128, :], [], [WOP.p[g]])
        for h in range(8):
            kb.dma("pool", WOA.t[:, h, :], P["pa_w_out"][jl, 512 + h * 64:512 + (h + 1) * 64, :], [], [WOA.p[h]])
        kb.dma("pool", CMB.t[:, :, :], P["c_cmb"].rearrange("i p c -> p i c"), [], [CMB.res])
        for i in range(2):
            kb.op("pool", [], [VS[i].res], lambda e, i=i: e.memset(VS[i].t[:, :, 64:65], 1.0))

        def tile_prologue(n):
            def f():
                t0 = n * TS
                Xn, Pn = X[n % 2], PAg[n % 2]
                kb.dma("sp", Xn.t[:, :, :], x_src[:, t0:t0 + TS].rearrange("(c p) t -> p c t", p=128), [], [Xn.res])
                kb.dma("sp", Pn.t[:, :, :], pad[:, t0:t0 + TS].rearrange("(g p) t -> p g t", p=128), r_p[n], [Pn.res])
            return {0: f}

        def head_prologue(n, h):
            def f():
                t0 = n * TS
                nk = 4 * n + 4
                b = (n * 8 + h) % 2
                kb.dma("sp", QP[b].t[:, :], qd[h, :, t0:t0 + TS], r_q[n] + aux, [QP[b].res])
                kb.dma("sp", KP[b].t[:, 0:nk * 128], kd[h, :, 0:nk * 128], cat(r_k[0:n + 1]) + aux, [KP[b].res])
                kb.dma("sp", VS[b].t[:, 0:nk, 0:64], vd[0:nk * 128, h * 64:(h + 1) * 64].rearrange("(k p) d -> p k d", p=128),
                       cat(r_v[0:n + 1]), [VS[b].res])
            return {3: f}

        kcnt = [0]

        def kt_chain(n, h, kt):
            k = kcnt[0]
            kcnt[0] += 1
            nk = 4 * n + 4
            b = (n * 8 + h) % 2
            acc = ps_acc[(n * 8 + h) % 2]
            ps, pt = ps_s[k % 3], PT[k % 4]
            diag = kt >= 4 * n

            def s0():
                kb.op("pe", [KP[b].res, QP[b].res], [ps.res],
                      lambda e: e.matmul(ps.t[:, :], KP[b].t[:, kt * 128:(kt + 1) * 128], QP[b].t[:, :], start=True, stop=not diag))
                if diag:
                    kb.op("pe", [CMB.res, cst["ident"].res], [ps.res],
                          lambda e: e.matmul(ps.t[:, :], cst["ident"].t[:, :], CMB.t[:, kt - 4 * n, :], start=False, stop=True))

            def s1():
                kb.op("act", [ps.res], [pt.res], lambda e: e.activation(out=pt.t[:, :], in_=ps.t[:, :], func=AF.Exp))

            def s3():
                kb.op("pe", [VS[b].res, pt.res], [acc.res],
                      lambda e: e.matmul(acc.t[:, :], VS[b].t[:, kt, :], pt.t[:, :], start=(kt == 0), stop=(kt == nk - 1)))

            return {0: s0, 1: s1, 3: s3}

        def head_epilogue(n, h):
            acc = ps_acc[(n * 8 + h) % 2]
            att = ATT[n % 2][h]

            def f1():
                kb.op("act", [acc.res], [RD.res], lambda e: e.copy(out=RD.t[64:65, :], in_=acc.t[64:65, :]))
                kb.op("pe", [RD.res, cst["onesf"].res], [ps_b.res],
                      lambda e: e.matmul(ps_b.t[:, :], cst["onesf"].t[64:65, 0:64], RD.t[64:65, :], start=True, stop=True))

            def f2():
                kb.op("dve", [ps_b.res], [BC.res], lambda e: e.reciprocal(out=BC.t[:, :], in_=ps_b.t[:, :]))
                kb.op("dve", [acc.res, BC.res], [att.res],
                      lambda e: e.tensor_tensor(out=att.t[:, :], in0=acc.t[0:64, :], in1=BC.t[:, :], op=ALU.mult))
            return {4: f1, 5: f2}

        def tile_epilogue(n):
            def f():
                t0 = n * TS
                Xn, Pn = X[n % 2], PAg[n % 2]
                for m in range(NC_):
                    po = ps_o[m % 2]
                    for g in range(4):
                        kb.op("pe", [WOP.p[g], Pn.res], [po.res],
                              lambda e, m=m, g=g, po=po: e.matmul(po.t[:, :], WOP.t[:, g, m * 128:(m + 1) * 128], Pn.t[:, g, :], start=(g == 0), stop=False))
                    for h in range(8):
                        att = ATT[n % 2][h]
                        kb.op("pe", [WOA.p[h], att.res], [po.res],
                              lambda e, m=m, h=h, att=att, po=po: e.matmul(po.t[:, :], WOA.t[:, h, m * 128:(m + 1) * 128], att.t[:, :], start=False, stop=(h == 7)))
                    kb.op("dve", [po.res, Xn.res], [Xn.res],
                          lambda e, m=m, po=po: e.tensor_tensor(out=Xn.t[:, m, :], in0=po.t[:, :], in1=Xn.t[:, m, :], op=ALU.add))
                kb.dma("sp", x_dst[:, t0:t0 + TS].rearrange("(c p) t -> p c t", p=128), Xn.t[:, :, :], [Xn.res], [])
            return {7: f}

        chains = []
        heads = [(n, h) for n in range(NT) for h in range(8)]
        chains.append(head_prologue(*heads[0]))
        for idx, (n, h) in enumerate(heads):
            if h == 0:
                chains.append(tile_prologue(n))
            if idx + 1 < len(heads):
                chains.append(head_prologue(*heads[idx + 1]))
            for kt in range(4 * n + 4):
                chains.append(kt_chain(n, h, kt))
            chains.append(head_epilogue(n, h))
            if h == 7:
                chains.append(tile_epilogue(n))
        emit_skewed(chains)


def make_consts(T):
    NT = T // TS
    t = np.arange(T)
    slopes = 2.0 ** (-(np.arange(8) + 1.0))
    a, b = (t // 64).astype(np.float32), (t % 64).astype(np.float32)
    kaux = np.zeros((8, 20, T), np.float32)
    qaux = np.zeros((8, 4, T), np.float32)
    for h in range(8):
        kaux[h, (t // 256), t] = 1.0
        kaux[h, 16] = slopes[h] * 64 * a
        kaux[h, 17] = slopes[h] * b
        kaux[h, 18] = 1.0
        kaux[h, 19] = 1.0
        qaux[h, 0] = 1.0
        qaux[h, 1] = 1.0
        qaux[h, 2] = -slopes[h] * 64 * a
        qaux[h, 3] = -slopes[h] * b
    pbinf = np.zeros((NT, 128, 4, 8, 16), np.float32)
    pb30k = np.zeros((NT, 128, 4, 8, 16), np.float32)
    blk = np.arange(16)
    for n in range(NT):
        for sub in range(4):
            bq = 2 * n + sub // 2
            pbinf[n, :, sub, :, :] = np.where(blk < bq, 0.0, np.where(blk == bq, 1e30, -1e30))
            pb30k[n, :, sub, :, :] = np.where(blk <= bq, NEG, 2 * NEG)
    rc = np.zeros((128, 4, TS), np.float32)
    for g in range(4):
        rc[:, g, :] = 1.0 / np.minimum(np.arange(TS) + 1.0, 2.0 ** (g + 1))
    cmb = np.zeros((4, 128, TS), np.float32)
    pp = np.arange(128)[:, None]
    cc = np.arange(TS)[None, :]
    for i in range(4):
        cmb[i] = np.where(cc >= i * 128 + pp, 0.0, NEG)
    bones = np.zeros((128, 128), np.float32)
    bones[:64, :64] = 1.0
    bones[64:, 64:] = 1.0
    return {"c_kaux": kaux, "c_qaux": qaux, "c_pbinf": pbinf.reshape(NT, 128, TS), "c_pb30k": pb30k.reshape(NT, 128, TS),
            "c_rc": rc, "c_cmb": cmb, "c_ident": np.eye(128, dtype=np.float32), "c_bones": bones}


def _pc(v, p=128):
    v = np.asarray(v, dtype=np.float32)
    return np.ascontiguousarray(np.swapaxes(v.reshape(*v.shape[:-1], v.shape[-1] // p, p), -1, -2))


DEV_SPECS = {
    "norm_mix_t": ((4, 128, 8), "mix"), "norm_ffn_t": ((4, 128, 8), "ffn"),
    "ffn_w_in": ((4, 1024, 5632), "ffn"), "ffn_w_out": ((4, 2816, 1024), "ffn"),
    "ffn_cw": ((4, 128, 3, 44), "ffn"), "ffn_cb": ((4, 128, 44), "ffn"),
    "rg_w_in": ((2, 1024, 2560), "rg"), "rg_w_out": ((2, 1280, 1024), "rg"),
    "rg_w_a": ((2, 10, 128, 128), "rg"), "rg_w_x": ((2, 10, 128, 128), "rg"),
    "rg_cw": ((2, 128, 4, 10), "rg"), "rg_vec": ((2, 128, 4, 10), "rg"),
    "pa_w_in": ((2, 1024, 2048), "pa"), "pa_w_out": ((2, 1024, 1024), "pa"), "pa_pool_w": ((2, 4, 128, 128), "pa"),
    "pa_vec": ((2, 128, 6), "pa"),
}


def const_specs(T):
    NT = T // TS
    return {"c_kaux": (8, 20, T), "c_qaux": (8, 4, T), "c_pbinf": (NT, 128, TS), "c_pb30k": (NT, 128, TS),
            "c_rc": (128, 4, TS), "c_cmb": (4, 128, TS), "c_ident": (128, 128), "c_bones": (128, 128)}


def prep_inputs(inputs):
    f = lambda k: np.ascontiguousarray(inputs[k], dtype=np.float32)
    d = {}
    d["norm_mix_t"] = _pc(f("norm_mix"))
    d["norm_ffn_t"] = _pc(f("norm_ffn"))
    for k in ("ffn_w_in", "ffn_w_out", "rg_w_in", "rg_w_out", "rg_w_a", "rg_w_x", "pa_w_in", "pa_w_out", "pa_pool_w"):
        d[k] = f(k)
    d["ffn_cw"] = np.ascontiguousarray(np.transpose(_pc(f("ffn_conv_w")), (0, 2, 1, 3)))
    d["ffn_cb"] = _pc(f("ffn_conv_b"))
    d["rg_cw"] = np.ascontiguousarray(np.transpose(_pc(f("rg_conv_w")), (0, 2, 1, 3)))
    d["rg_vec"] = np.ascontiguousarray(np.stack([_pc(f("rg_conv_b")), _pc(f("rg_b_a")), _pc(f("rg_b_x")), _pc(f("rg_lambda"))], axis=2))
    qg = np.concatenate([f("pa_q_gain"), f("pa_q_gain")], axis=1)
    kg = np.concatenate([f("pa_k_gain"), f("pa_k_gain")], axis=1)
    d["pa_vec"] = np.ascontiguousarray(np.concatenate([_pc(f("pa_pool_scale")), qg[:, :, None], kg[:, :, None]], axis=2))
    return d


def build(T=4096, plan=None):
    if plan is None:
        plan = []
        for l in range(4):
            plan.append(("pa" if l % 2 == 0 else "rg", l))
            plan.append(("ffn", l))
    nc = bass.Bass("TRN2", target_bir_lowering=False)
    xT = nc.dram_tensor("xT", [D, T], F32, kind="ExternalInput").ap()
    yT = nc.dram_tensor("yT", [D, T], F32, kind="ExternalOutput").ap()
    kinds = {k for k, _ in plan}
    if "pa" in kinds or "rg" in kinds:
        kinds.add("mix")
    used = [k for k, (shp, grp) in DEV_SPECS.items() if grp in kinds]
    P = {k: nc.dram_tensor(k, list(DEV_SPECS[k][0]), F32, kind="ExternalInput").ap() for k in used}
    scrd = {}
    if "ffn" in kinds:
        scrd["w2s"] = nc.dram_tensor("w2s", [NC_, 128, NJ, 128], BF16).ap()
    if "pa" in kinds:
        for k, shp in const_specs(T).items():
            P[k] = nc.dram_tensor(k, list(shp), F32, kind="ExternalInput").ap()
            used.append(k)
        scrd["qd"] = nc.dram_tensor("qd", [8, 84, T], BF16).ap()
        scrd["kd"] = nc.dram_tensor("kd", [8, 84, T], BF16).ap()
        scrd["vd"] = nc.dram_tensor("vd", [T, 512], BF16).ap()
        scrd["pad"] = nc.dram_tensor("pad", [512, T], BF16).ap()
    scr = [nc.dram_tensor(f"xs{i}", [D, T], F32).ap() for i in range(2)]
    with ExitStack() as es:
        kb = KB(nc, es)
        cst = {}
        cst["ones"] = alloc(es, nc, "c_ones", [128, 128], BF16)
        cst["eps"] = alloc(es, nc, "c_eps", [128, 1], F32)
        kb.op("pool", [], [cst["ones"].res], lambda e: e.memset(cst["ones"].t[:, :], 1.0))
        kb.op("pool", [], [cst["eps"].res], lambda e: e.memset(cst["eps"].t[:, :], EPS))
        cst["one"] = alloc(es, nc, "c_one", [128, 1], F32)
        kb.op("pool", [], [cst["one"].res], lambda e: e.memset(cst["one"].t[:, :], 1.0))
        if "pa" in kinds:
            cst["ident"] = alloc(es, nc, "c_identb", [128, 128], BF16)
            cst["bones"] = alloc(es, nc, "c_bonesb", [128, 128], BF16)
            cst["onesf"] = alloc(es, nc, "c_onesf", [65, 64], F32)
            kb.dma("pool", cst["ident"].t[:, :], P["c_ident"], [], [cst["ident"].res])
            kb.dma("pool", cst["bones"].t[:, :], P["c_bones"], [], [cst["bones"].res])
            kb.op("pool", [], [cst["onesf"].res], lambda e: e.memset(cst["onesf"].t[:, :], 1.0))
            scrd["aux_res"] = []
            for h in range(8):
                r1, r2 = Res(), Res()
                kb.dma("pool", scrd["kd"][h, 64:84, :], P["c_kaux"][h], [], [r1])
                kb.dma("pool", scrd["qd"][h, 80:84, :], P["c_qaux"][h], [], [r2])
                scrd["aux_res"] += [r1, r2]
        src = xT
        for i, (kind, l) in enumerate(plan):
            if i > 0:
                kb.barrier()
            dst = yT if i == len(plan) - 1 else scr[i % 2]
            if kind == "ffn":
                ffn_pass(kb, P, l, src, dst, T, cst, scrd)
            elif kind == "rg":
                rg_pass(kb, P, l, src, dst, T, cst)
            elif kind == "pa":
                pa_pass(kb, P, l, src, dst, T, cst, scrd)
            else:
                raise NotImplementedError(kind)
            src = dst
        for q, ring in kb.rings.items():
            for tok in ring["last"]:
                if tok is not None:
                    kb._wait("sp", tok)
        for e in ("pe", "act", "dve", "pool"):
            if kb.cnt[e]:
                kb._wait("sp", (e, kb.sem[e], kb.cnt[e]))
    kb.used = used
    return nc, kb


def kernel(**inputs):
    x = np.ascontiguousarray(inputs["x"], dtype=np.float32)
    B, S, _ = x.shape
    nc, kb = build(T=S)
    dev = prep_inputs(inputs)
    dev.update(make_consts(S))
    params = {k: dev[k] for k in kb.used}
    in_maps = []
    for b in range(B):
        m = dict(params)
        m["xT"] = np.ascontiguousarray(x[b].T)
        in_maps.append(m)
    res = run_bass_kernel_spmd(nc, in_maps, core_ids=list(range(B)))
    out = np.stack([np.ascontiguousarray(res.results[b]["yT"].T) for b in range(B)], axis=0)
    return out.astype(np.float32)
```
